# Optimizing a Trainium2 kernel written in Bass

```python
import math
import jax, jax.numpy as jnp
from jax import lax
import numpy as np

D_MODEL = 1024
BATCH = 8
SEQ = 8192
DEPTH = 1

GRID_W = 64
CTX_LEN = 256
N_HEADS = 8
QK_NOPE = 64
QK_ROPE = 32
V_HEAD = 64
Q_LORA = 256
KV_LORA = 128
ROPE_THETA = 10000.0
Q_BLOCK = 128
ATTN_SCALE = 1.0 / math.sqrt(QK_NOPE + QK_ROPE)
HY_WIDTH = 512
HY_ORDER = 2
HY_SHORT = 3
HY_BANDS = 8
HY_EMB = 1 + 2 * HY_BANDS
HY_FILTER_HIDDEN = 64
HY_FAST_DECAY = 0.3
HY_SLOW_DECAY = 1.5
HY_DECAY_TARGET = 1e-2
N_EXPERTS = 64
N_GROUPS = 8
TOPK_GROUPS = 4
TOP_K = 8
EXPERT_FF = 256
SHARED_FF = 256
ROUTE_SCALE = 2.5
EXPERT_BLOCK = 512
NORM_EPS = 1e-6
IN_WIDTH = Q_LORA + KV_LORA + QK_ROPE + 3 * HY_WIDTH + 2 * D_MODEL

kernel_name = "hybrid_mla_hyena_moe_diffusion_block"


def rmsnorm(x, g):
    xf = x.astype(jnp.float32)
    y = xf * lax.rsqrt(jnp.mean(xf * xf, axis=-1, keepdims=True) + NORM_EPS)
    return (y * g.astype(jnp.float32)).astype(x.dtype)


def modulate(x, shift, scale):
    return x * (1 + scale) + shift


def _rotate(x, ang):
    xf = x.astype(jnp.float32)
    x1, x2 = jnp.split(xf, 2, axis=-1)
    cos, sin = jnp.cos(ang), jnp.sin(ang)
    return jnp.concatenate([x1 * cos - x2 * sin, x1 * sin + x2 * cos], axis=-1).astype(x.dtype)


def axial_angles(n_tokens):
    rows = n_tokens // GRID_W
    row = jnp.broadcast_to(jnp.arange(rows, dtype=jnp.float32)[:, None], (rows, GRID_W)).reshape(-1)
    col = jnp.broadcast_to(jnp.arange(GRID_W, dtype=jnp.float32)[None, :], (rows, GRID_W)).reshape(-1)
    half = QK_ROPE // 2
    inv_freq = ROPE_THETA ** (-jnp.arange(0, half, 2, dtype=jnp.float32) / half)
    return row[:, None] * inv_freq, col[:, None] * inv_freq


def rope2d(x, ang_row, ang_col):
    half = QK_ROPE // 2
    return jnp.concatenate([_rotate(x[..., :half], ang_row), _rotate(x[..., half:], ang_col)], axis=-1)


def split_proj(p):
    cuts = [Q_LORA, Q_LORA + KV_LORA, Q_LORA + KV_LORA + QK_ROPE,
            Q_LORA + KV_LORA + QK_ROPE + 3 * HY_WIDTH]
    return jnp.split(p, cuts, axis=-1)


def mla_q(q_lat, q_norm, w_uq):
    B, L, _ = q_lat.shape
    q = (rmsnorm(q_lat, q_norm) @ w_uq).reshape(B, L, N_HEADS, QK_NOPE + QK_ROPE)
    return q[..., :QK_NOPE], q[..., QK_NOPE:]


def mla_kv(kv_lat, kv_norm, w_ukv):
    B, L, _ = kv_lat.shape
    kv = (rmsnorm(kv_lat, kv_norm) @ w_ukv).reshape(B, L, N_HEADS, QK_NOPE + V_HEAD)
    return kv[..., :QK_NOPE], kv[..., QK_NOPE:]


def attend(qn, qr, kn, kr, v):
    s = jnp.einsum('bqhd,bkhd->bhqk', qn, kn) + jnp.einsum('bqhr,bkr->bhqk', qr, kr)
    p = jax.nn.softmax(s.astype(jnp.float32) * ATTN_SCALE, axis=-1).astype(v.dtype)
    return jnp.einsum('bhqk,bkhd->bqhd', p, v)


def blocked_attention(qn, qr, kn, kr, v):
    B, S = qn.shape[:2]
    nb = S // Q_BLOCK

    def to_blocks(t):
        return jnp.moveaxis(t.reshape(B, nb, Q_BLOCK, *t.shape[2:]), 1, 0)

    out = lax.map(lambda qb: attend(qb[0], qb[1], kn, kr, v), (to_blocks(qn), to_blocks(qr)))
    return jnp.moveaxis(out, 0, 1).reshape(B, S, N_HEADS * V_HEAD)


def short_conv(u, w, b):
    L = u.shape[1]
    pad = HY_SHORT // 2
    up = jnp.pad(u, ((0, 0), (pad, HY_SHORT - 1 - pad), (0, 0)))
    return sum(up[:, k:k + L] * w[k] for k in range(HY_SHORT)) + b


def hyena_filters(L, w1, b1, w2, b2, w3, freq):
    t = jnp.linspace(0.0, 1.0, L, dtype=jnp.float32)[:, None]
    w = 2.0 * math.pi * jnp.arange(L, dtype=jnp.float32)[:, None] / L
    f = jnp.linspace(1e-4, HY_BANDS - 1, HY_BANDS, dtype=jnp.float32)[None, :]
    emb = jnp.concatenate([t, jnp.cos(f * w), -jnp.sin(f * w)], axis=-1)
    fr = freq.astype(jnp.float32)
    h = jnp.sin(fr * (emb @ w1.astype(jnp.float32) + b1.astype(jnp.float32)))
    h = jnp.sin(fr * (h @ w2.astype(jnp.float32) + b2.astype(jnp.float32)))
    k = (h @ w3.astype(jnp.float32)).reshape(L, 2 * HY_ORDER, HY_WIDTH)
    deltas = jnp.abs(jnp.linspace(math.log(HY_DECAY_TARGET) / HY_SLOW_DECAY,
                                  math.log(HY_DECAY_TARGET) / HY_FAST_DECAY, HY_WIDTH, dtype=jnp.float32))
    k = k * jnp.exp(-t * deltas)[:, None, :]
    fwd, bwd = k[:, 0::2], k[:, 1::2]
    full = jnp.concatenate([fwd, jnp.zeros_like(fwd[:1]), bwd[:0:-1]], axis=0)
    full = full / (jnp.sum(jnp.abs(full), axis=0, keepdims=True) + 1e-6)
    return jnp.fft.rfft(full, axis=0)


def fft_long_conv(u, kf, skip):
    L = u.shape[1]
    uf = jnp.fft.rfft(u.astype(jnp.float32), n=2 * L, axis=1)
    y = jnp.fft.irfft(uf * kf[None], n=2 * L, axis=1)[:, :L]
    return (y + u.astype(jnp.float32) * skip.astype(jnp.float32)).astype(u.dtype)


def hyena(u, kf, conv_w, conv_b, skip):
    u = short_conv(u, conv_w, conv_b)
    v, x1, x2 = jnp.split(u, 3, axis=-1)
    z = x1 * fft_long_conv(v, kf[:, 0], skip[0])
    return x2 * fft_long_conv(z, kf[:, 1], skip[1])


def merge_branches(attn_out, hy_out, gate, w_ba, w_bh, w_out):
    ga, gh = jnp.split(gate, 2, axis=-1)
    y = jax.nn.sigmoid(ga) * (attn_out @ w_ba) + jax.nn.sigmoid(gh) * (hy_out @ w_bh)
    return y @ w_out


def route(t, w_router, router_bias):
    T = t.shape[0]
    scores = jax.nn.sigmoid(t.astype(jnp.float32) @ w_router.astype(jnp.float32))
    sel = scores + router_bias.astype(jnp.float32)
    group_score = lax.top_k(sel.reshape(T, N_GROUPS, N_EXPERTS // N_GROUPS), 2)[0].sum(-1)
    _, gidx = lax.top_k(group_score, TOPK_GROUPS)
    gmask = jax.nn.one_hot(gidx, N_GROUPS, dtype=jnp.float32).sum(1)
    emask = jnp.repeat(gmask, N_EXPERTS // N_GROUPS, axis=1) > 0
    _, eidx = lax.top_k(jnp.where(emask, sel, -jnp.inf), TOP_K)
    w = jnp.take_along_axis(scores, eidx, axis=1)
    w = w / jnp.sum(w, axis=-1, keepdims=True) * ROUTE_SCALE
    return eidx, w


def routed_experts(t, eidx, w, wg, wu, wd):
    T, D = t.shape
    A = T * TOP_K
    flat_e = eidx.reshape(-1).astype(jnp.int32)
    flat_t = jnp.arange(A, dtype=jnp.int32) // TOP_K
    flat_w = w.reshape(-1)
    order = jnp.argsort(flat_e)
    se = flat_e[order]
    counts = jnp.bincount(flat_e, length=N_EXPERTS).astype(jnp.int32)
    padded = (counts + EXPERT_BLOCK - 1) // EXPERT_BLOCK * EXPERT_BLOCK
    pad_end = jnp.cumsum(padded)
    pad_start = pad_end - padded
    grp_start = jnp.cumsum(counts) - counts
    dest = pad_start[se] + (jnp.arange(A, dtype=jnp.int32) - grp_start[se])
    nblk = -(-A // EXPERT_BLOCK) + N_EXPERTS
    P = nblk * EXPERT_BLOCK
    buf_t = jnp.full((P,), T, jnp.int32).at[dest].set(flat_t[order])
    buf_w = jnp.zeros((P,), jnp.float32).at[dest].set(flat_w[order])
    blk_e = jnp.minimum(jnp.searchsorted(pad_end, jnp.arange(nblk, dtype=jnp.int32) * EXPERT_BLOCK,
                                         side='right'), N_EXPERTS - 1)
    t_pad = jnp.concatenate([t, jnp.zeros((1, D), t.dtype)], axis=0)

    def step(acc, blk):
        tok, wt, e = blk
        xb = t_pad[tok]
        a = jax.nn.silu(xb @ wg[e]) * (xb @ wu[e])
        out = (a @ wd[e]).astype(jnp.float32) * wt[:, None]
        return acc.at[tok].add(out), None

    acc, _ = lax.scan(step, jnp.zeros((T + 1, D), jnp.float32),
                      (buf_t.reshape(nblk, EXPERT_BLOCK), buf_w.reshape(nblk, EXPERT_BLOCK), blk_e))
    return acc[:T].astype(t.dtype)


def moe(h, w_router, router_bias, wg, wu, wd, wsg, wsu, wsd):
    B, L, D = h.shape
    t = h.reshape(B * L, D)
    eidx, w = route(t, w_router, router_bias)
    routed = routed_experts(t, eidx, w, wg, wu, wd)
    shared = (jax.nn.silu(t @ wsg) * (t @ wsu)) @ wsd
    return (routed + shared).reshape(B, L, D)


def setup_inputs(seed: int = 0) -> dict:
    key = jax.random.key(seed)
    ks = iter(jax.random.split(key, 40))

    def nrm(shape, scale):
        return jax.random.normal(next(ks), shape, jnp.float32) * scale

    def gain(shape):
        return 1.0 + 0.05 * jax.random.normal(next(ks), shape, jnp.float32)

    L_ = DEPTH
    return {
        "x": nrm((BATCH, SEQ, D_MODEL), 1.0),
        "c": nrm((BATCH, D_MODEL), 1.0),
        "ctx": nrm((BATCH, CTX_LEN, D_MODEL), 1.0),
        "c_ctx": nrm((D_MODEL,), 1.0),
        "w_mod": nrm((L_, D_MODEL, 6 * D_MODEL), 0.5 * D_MODEL ** -0.5),
        "b_mod": nrm((L_, 6 * D_MODEL), 0.02),
        "norm_mix": gain((L_, D_MODEL)),
        "norm_ffn": gain((L_, D_MODEL)),
        "w_in": nrm((L_, D_MODEL, IN_WIDTH), D_MODEL ** -0.5),
        "b_in": nrm((L_, IN_WIDTH), 0.02),
        "q_norm": gain((L_, Q_LORA)),
        "w_uq": nrm((L_, Q_LORA, N_HEADS * (QK_NOPE + QK_ROPE)), Q_LORA ** -0.5),
        "kv_norm": gain((L_, KV_LORA)),
        "w_ukv": nrm((L_, KV_LORA, N_HEADS * (QK_NOPE + V_HEAD)), KV_LORA ** -0.5),
        "w_branch_attn": nrm((L_, N_HEADS * V_HEAD, D_MODEL), (N_HEADS * V_HEAD) ** -0.5),
        "hy_conv_w": nrm((L_, HY_SHORT, 3 * HY_WIDTH), HY_SHORT ** -0.5),
        "hy_conv_b": nrm((L_, 3 * HY_WIDTH), 0.02),
        "hy_filt_w1": nrm((L_, HY_EMB, HY_FILTER_HIDDEN), HY_EMB ** -0.5),
        "hy_filt_b1": nrm((L_, HY_FILTER_HIDDEN), 0.1),
        "hy_filt_w2": nrm((L_, HY_FILTER_HIDDEN, HY_FILTER_HIDDEN), HY_FILTER_HIDDEN ** -0.5),
        "hy_filt_b2": nrm((L_, HY_FILTER_HIDDEN), 0.1),
        "hy_filt_w3": nrm((L_, HY_FILTER_HIDDEN, 2 * HY_ORDER * HY_WIDTH), HY_FILTER_HIDDEN ** -0.5),
        "hy_filt_freq": gain((L_, HY_FILTER_HIDDEN)),
        "hy_skip": nrm((L_, HY_ORDER, HY_WIDTH), 0.5),
        "w_branch_hyena": nrm((L_, HY_WIDTH, D_MODEL), HY_WIDTH ** -0.5),
        "w_out": nrm((L_, D_MODEL, D_MODEL), D_MODEL ** -0.5),
        "w_router": nrm((L_, D_MODEL, N_EXPERTS), D_MODEL ** -0.5),
        "router_bias": nrm((L_, N_EXPERTS), 0.01),
        "w_exp_gate": nrm((L_, N_EXPERTS, D_MODEL, EXPERT_FF), D_MODEL ** -0.5),
        "w_exp_up": nrm((L_, N_EXPERTS, D_MODEL, EXPERT_FF), D_MODEL ** -0.5),
        "w_exp_down": nrm((L_, N_EXPERTS, EXPERT_FF, D_MODEL), EXPERT_FF ** -0.5),
        "w_sh_gate": nrm((L_, D_MODEL, SHARED_FF), D_MODEL ** -0.5),
        "w_sh_up": nrm((L_, D_MODEL, SHARED_FF), D_MODEL ** -0.5),
        "w_sh_down": nrm((L_, SHARED_FF, D_MODEL), SHARED_FF ** -0.5),
        "final_norm": gain((D_MODEL,)),
    }


def reference(x, c, ctx, c_ctx, w_mod, b_mod, norm_mix, norm_ffn, w_in, b_in,
              q_norm, w_uq, kv_norm, w_ukv, w_branch_attn,
              hy_conv_w, hy_conv_b, hy_filt_w1, hy_filt_b1, hy_filt_w2, hy_filt_b2,
              hy_filt_w3, hy_filt_freq, hy_skip, w_branch_hyena, w_out,
              w_router, router_bias, w_exp_gate, w_exp_up, w_exp_down,
              w_sh_gate, w_sh_up, w_sh_down, final_norm):
    B, S, D = x.shape
    n_ctx = ctx.shape[1]
    ang_r, ang_c = axial_angles(S)
    cx = ctx
    for i in range(DEPTH):
        last = i == DEPTH - 1
        mod = (jax.nn.silu(c) @ w_mod[i] + b_mod[i]).reshape(B, 6, D)[:, :, None, :]
        modc = (jax.nn.silu(c_ctx) @ w_mod[i] + b_mod[i]).reshape(6, D)
        filt = (hy_filt_w1[i], hy_filt_b1[i], hy_filt_w2[i], hy_filt_b2[i], hy_filt_w3[i], hy_filt_freq[i])

        h = modulate(rmsnorm(x, norm_mix[i]), mod[:, 0], mod[:, 1])
        hc = modulate(rmsnorm(cx, norm_mix[i]), modc[0], modc[1])
        q_lat, kv_lat, kpe, hy_in, gate = split_proj(h @ w_in[i] + b_in[i])
        cq_lat, ckv_lat, ckpe, chy_in, cgate = split_proj(hc @ w_in[i] + b_in[i])

        qn, qr = mla_q(q_lat, q_norm[i], w_uq[i])
        qr = rope2d(qr, ang_r[:, None], ang_c[:, None])
        kn, v = mla_kv(kv_lat, kv_norm[i], w_ukv[i])
        kpe = rope2d(kpe, ang_r, ang_c)
        ckn, cv = mla_kv(ckv_lat, kv_norm[i], w_ukv[i])
        kn_all = jnp.concatenate([ckn, kn], axis=1)
        kr_all = jnp.concatenate([ckpe, kpe], axis=1)
        v_all = jnp.concatenate([cv, v], axis=1)
        attn = blocked_attention(qn, qr, kn_all, kr_all, v_all)

        hy_out = hyena(hy_in, hyena_filters(S, *filt), hy_conv_w[i], hy_conv_b[i], hy_skip[i])
        x_mixed = x + mod[:, 2] * merge_branches(attn, hy_out, gate, w_branch_attn[i],
                                                 w_branch_hyena[i], w_out[i])

        if not last:
            cqn, cqr = mla_q(cq_lat, q_norm[i], w_uq[i])
            c_attn = attend(cqn, cqr, ckn, ckpe, cv).reshape(B, n_ctx, N_HEADS * V_HEAD)
            c_hy = hyena(chy_in, hyena_filters(n_ctx, *filt), hy_conv_w[i], hy_conv_b[i], hy_skip[i])
            cx = cx + modc[2] * merge_branches(c_attn, c_hy, cgate, w_branch_attn[i],
                                               w_branch_hyena[i], w_out[i])
            hc2 = modulate(rmsnorm(cx, norm_ffn[i]), modc[3], modc[4])
            cx = cx + modc[5] * moe(hc2, w_router[i], router_bias[i], w_exp_gate[i], w_exp_up[i],
                                    w_exp_down[i], w_sh_gate[i], w_sh_up[i], w_sh_down[i])

        x = x_mixed
        h2 = modulate(rmsnorm(x, norm_ffn[i]), mod[:, 3], mod[:, 4])
        x = x + mod[:, 5] * moe(h2, w_router[i], router_bias[i], w_exp_gate[i], w_exp_up[i],
                                w_exp_down[i], w_sh_gate[i], w_sh_up[i], w_sh_down[i])
    return rmsnorm(x, final_norm)
```

```python
import math
from contextlib import ExitStack
import numpy as np
import concourse.bass as bass
import concourse.mybir as mybir
from concourse.bass_utils import run_bass_kernel_spmd

F32 = mybir.dt.float32
BF16 = mybir.dt.bfloat16
AF = mybir.ActivationFunctionType
ALU = mybir.AluOpType
AX = mybir.AxisListType

ENGS = ("tensor", "vector", "scalar", "gpsimd", "sync")

D = 1024
SEQ = 8192
NCTX = 256
NKEY = SEQ + NCTX
H = 8
DQ = 96
HYW = 512
NE = 64
EFF = 256
NFFT = 2 * SEQ
EPS = 1e-6
ATTN_SCALE = 1.0 / math.sqrt(96.0)
IN_WIDTH = 4000
C_Q, C_KV, C_KPE, C_HY, C_GATE = 0, 256, 384, 416, 1952


class Buf:
    __slots__ = ("name", "t", "last_w", "readers", "dsem")

    def __init__(self, name, t=None):
        self.name = name
        self.t = t
        self.last_w = None
        self.readers = []
        self.dsem = {}

    def __getitem__(self, k):
        return self.t[k]


class Sync:
    def __init__(self, nc):
        self.nc = nc
        self.lists = {e: [] for e in ENGS}
        self.cnt = {e: 0 for e in ENGS}
        self.esem = {e: nc.alloc_semaphore("es_" + e) for e in ENGS}
        self.known = {e: {} for e in ENGS}
        self.semvals = {}
        self.free_dsems = {"hw": [], "sw": []}
        self.n_dsems = 0
        self.ninst = 0
        for e in ENGS:
            self.semvals[id(self.esem[e])] = [self.esem[e], 0]

    def _wait(self, eng, tok):
        sem, val = tok
        k = self.known[eng]
        if k.get(id(sem), 0) >= val:
            return
        k[id(sem)] = val
        self.lists[eng].append(lambda e, sem=sem, val=val: e.wait_ge(sem, val))

    def _deps(self, eng, r, w):
        toks = []
        for b in r:
            if b.last_w is not None:
                toks.append(b.last_w)
        for b in w:
            if b.last_w is not None:
                toks.append(b.last_w)
            toks.extend(b.readers)
        pe = self.esem["tensor"]
        best = {}
        for sem, val in toks:
            if eng == "tensor" and sem is pe:
                continue
            if best.get(id(sem), (None, -1))[1] < val:
                best[id(sem)] = (sem, val)
        for tok in best.values():
            self._wait(eng, tok)

    def _mark(self, tok, r, w):
        for b in r:
            rd = b.readers
            rd.append(tok)
            if len(rd) > 16:
                best = {}
                for s, v in rd:
                    if best.get(id(s), (None, -1))[1] < v:
                        best[id(s)] = (s, v)
                b.readers = list(best.values())
        for b in w:
            b.last_w = tok
            b.readers = []

    def op(self, eng, fn, r=(), w=(), inc=True):
        self._deps(eng, r, w)
        sem = self.esem[eng]
        self.ninst += 1
        if inc:
            self.cnt[eng] += 1
            v = self.cnt[eng]
            self.semvals[id(sem)][1] = v
            self.lists[eng].append(lambda e, fn=fn, sem=sem: fn(e).then_inc(sem, 1))
            tok = (sem, v)
        else:
            self.lists[eng].append(lambda e, fn=fn: fn(e))
            tok = (sem, self.cnt[eng] + 1)
        self._mark(tok, r, w)
        return tok

    def _dsem(self, b, q):
        kind = "sw" if q == "gpsimd" else "hw"
        if kind not in b.dsem:
            if self.free_dsems[kind]:
                b.dsem[kind] = self.free_dsems[kind].pop()
            else:
                self.n_dsems += 1
                sem = self.nc.alloc_semaphore("ds%d" % self.n_dsems)
                b.dsem[kind] = sem
                self.semvals[id(sem)] = [sem, 0]
        return b.dsem[kind]

    def dma(self, q, out, in_, r=(), w=(), key=None, **kw):
        self._deps(q, r, w)
        sem = self._dsem(key, q)
        sv = self.semvals[id(sem)]
        sv[1] += 16
        v = sv[1]
        self.ninst += 1
        self.lists[q].append(
            lambda e, out=out, in_=in_, sem=sem, kw=kw: e.dma_start(out=out, in_=in_, **kw).then_inc(sem, 16))
        tok = (sem, v)
        self._mark(tok, r, w)
        return tok

    def release(self, bufs):
        for b in bufs:
            for kind, sem in b.dsem.items():
                self.free_dsems[kind].append(sem)
            b.dsem = {}

    def barrier(self):
        for e in ENGS:
            for sem, v in list(self.semvals.values()):
                if v > 0:
                    self._wait(e, (sem, v))

    def emit(self):
        nc = self.nc
        lists = self.lists
        with nc.Block() as block:
            @block.tensor
            def _(e):
                for f in lists["tensor"]:
                    f(e)

            @block.vector
            def _(e):
                for f in lists["vector"]:
                    f(e)

            @block.scalar
            def _(e):
                for f in lists["scalar"]:
                    f(e)

            @block.gpsimd
            def _(e):
                for f in lists["gpsimd"]:
                    f(e)

            @block.sync
            def _(e):
                for f in lists["sync"]:
                    f(e)
        self.lists = {e: [] for e in ENGS}


class Rot:
    def __init__(self, bufs):
        self.bufs = bufs
        self.i = 0

    def next(self):
        b = self.bufs[self.i % len(self.bufs)]
        self.i += 1
        return b


class KB:
    def __init__(self, dbg=(), dbg_in=()):
        self.nc = bass.Bass("TRN2", target_bir_lowering=False)
        self.S = Sync(self.nc)
        self.dbg = set(dbg)
        self.dbg_in = set(dbg_in)
        self.es = None
        self.phase_bufs = []
        self.inputs = {}
        self.uid = 0
        nc = self.nc
        self.P = nc.alloc_psum_tensor("P", [128, 4096], F32).ap()
        self.Pb = self.P.bitcast(BF16)
        self.pb = [Buf("pb%d" % i, self.P[:, 512 * i:512 * (i + 1)]) for i in range(8)]
        self.pbb = [self.Pb[:, 1024 * i:1024 * (i + 1)] for i in range(8)]

    def inp(self, name, shape, dt=F32):
        t = self.nc.dram_tensor(name, list(shape), dt, kind="ExternalInput").ap()
        self.inputs[name] = t
        return t

    def scratch(self, name, shape, dt):
        kind = "ExternalOutput" if name in self.dbg else "Internal"
        if name in self.dbg_in:
            kind = "ExternalInput"
        t = self.nc.dram_tensor(name, list(shape), dt, kind=kind).ap()
        if name in self.dbg_in:
            self.inputs[name] = t
        return t

    def sb(self, name, shape, dt, persist=False):
        self.uid += 1
        nm = "%s_%d" % (name, self.uid)
        if persist:
            t = self.nc.alloc_sbuf_tensor(nm, list(shape), dt).ap()
            return Buf(nm, t)
        t = self.es.enter_context(self.nc.sbuf_tensor(nm, list(shape), dt))
        b = Buf(nm, t.ap() if hasattr(t, "ap") and callable(getattr(t, "ap")) else t)
        self.phase_bufs.append(b)
        return b

    def rot(self, name, n, shape, dt):
        return Rot([self.sb("%s%d" % (name, i), shape, dt) for i in range(n)])

    def begin_phase(self):
        self.es = ExitStack()
        self.phase_bufs = []

    def end_phase(self):
        self.S.barrier()
        self.S.emit()
        self.S.release(self.phase_bufs)
        self.es.close()
        self.es = None


def _v(eng):
    return eng


def build(dbg=(), phases=("p0", "p1", "attn", "filt", "hyena", "merge", "moe"), dbg_in=(), lim=None):
    kb = KB(dbg, dbg_in)
    lim = lim or {}
    nc, S = kb.nc, kb.S
    pb = kb.pb
    op, dma = S.op, S.dma

    x_d = kb.inp("x", [SEQ, D])
    ctx_d = kb.inp("ctx", [NCTX, D])
    cT_d = kb.inp("cT", [128, 8])
    cctxT_d = kb.inp("cctxT", [128, 8])
    wmod_d = kb.inp("w_mod", [D, 6 * D])
    bmod_d = kb.inp("b_mod", [1, 6 * D])
    nmix_d = kb.inp("nmix_b", [128, D])
    nffn_d = kb.inp("nffn_b", [128, D])
    fn_d = kb.inp("fn_b", [128, D])
    win_d = kb.inp("w_in", [D, IN_WIDTH])
    binT_d = kb.inp("b_inT", [128, 33])
    wkpesw_d = kb.inp("w_kpe_sw", [D, 32])
    ropeC_d = kb.inp("ropeC", [32, SEQ])
    ropeS_d = kb.inp("ropeS", [32, SEQ])
    qnormT_d = kb.inp("q_normT", [128, 2])
    kvnormT_d = kb.inp("kv_normT", [128, 1])
    wuq_d = kb.inp("w_uq", [256, H * DQ])
    wuqsw_d = kb.inp("w_uq_sw", [256, H * DQ])
    wukvk_d = kb.inp("w_ukv_k", [128, 512])
    wukvv_d = kb.inp("w_ukv_v", [128, 512])
    hcw_d = kb.inp("hy_conv_wT", [128, 36])
    hcb_d = kb.inp("hy_conv_bT", [128, 12])
    ident_d = kb.inp("ident", [128, 128])

    MODS = kb.scratch("MODS", [8, 128, D], F32)
    Qs = kb.scratch("Qs", [H, DQ, SEQ], BF16)
    Ks = kb.scratch("Ks", [H, DQ, NKEY], BF16)
    Vs = kb.scratch("Vs", [NKEY, 512], BF16)
    Us = kb.scratch("Us", [1536, SEQ + 128], BF16)
    Gs = kb.scratch("Gs", [2048, SEQ], BF16)

    ident = kb.sb("ident", [128, 128], F32, persist=True)
    identb = kb.sb("identb", [128, 128], BF16, persist=True)
    ones = kb.sb("ones", [128, 128], F32, persist=True)
    onesb = kb.sb("onesb", [128, 128], BF16, persist=True)
    prot = Rot(pb)

    kb.begin_phase()
    dma("sync", ident[:], ident_d, w=[ident], key=ident)
    dma("gpsimd", identb[:], ident_d, w=[identb], key=identb)
    op("vector", lambda e: e.memset(ones[:], 1.0), w=[ones])
    op("vector", lambda e: e.memset(onesb[:], 1.0), w=[onesb])

    if "p0" in phases:
        cT = kb.sb("cT", [128, 16], F32)
        sc = kb.sb("sc", [128, 16], F32)
        scb = kb.sb("scb", [128, 16, 128], F32)
        bmod = kb.sb("bmod", [1, 6 * D], F32)
        nmix = kb.sb("nmix", [128, D], F32)
        nffn = kb.sb("nffn", [128, D], F32)
        wmr = kb.rot("wm", 2, [128, 8, 512], F32)
        mtr = kb.rot("mt", 3, [128, 512], F32)
        dma("sync", cT[:, 0:8], cT_d, w=[cT], key=cT)
        dma("sync", cT[:, 8:16], cctxT_d, w=[cT], key=cT)
        dma("sync", bmod[:], bmod_d, w=[bmod], key=bmod)
        dma("sync", nmix[:], nmix_d, w=[nmix], key=nmix)
        dma("sync", nffn[:], nffn_d, w=[nffn], key=nffn)
        op("scalar", lambda e: e.activation(out=sc[:], in_=cT[:], func=AF.Silu), r=[cT], w=[sc])
        op("vector", lambda e: e.tensor_copy(out=scb[:], in_=sc[:].unsqueeze(2).to_broadcast([128, 16, 128])), r=[sc], w=[scb])
        wm_v = wmod_d.rearrange("(k p) n -> p k n", p=128)
        for n in range(12):
            wm = wmr.next()
            dma("sync", wm[:], wm_v[:, :, n * 512:(n + 1) * 512], w=[wm], key=wm)
            j, half = n // 2, n % 2
            cs = slice(half * 512, half * 512 + 512)
            for which in range(2 if n < 4 else 1):
                bank = prot.next()
                for k in range(8):
                    op("tensor", lambda e, bank=bank, k=k, wm=wm, which=which: e.matmul(
                        bank[:], lhsT=scb[:, which * 8 + k, :], rhs=wm[:, k, :], start=(k == 0), stop=False),
                       r=[scb, wm], w=[bank], inc=False)
                op("tensor", lambda e, bank=bank, n=n: e.matmul(
                    bank[:], lhsT=ones[0:1, :], rhs=bmod[0:1, n * 512:(n + 1) * 512], start=False, stop=True),
                   r=[ones, bmod], w=[bank])
                mt = mtr.next()
                if j in (1, 4):
                    gb = nmix if j == 1 else nffn
                    op("vector", lambda e, mt=mt, bank=bank, gb=gb, cs=cs: e.scalar_tensor_tensor(
                        out=mt[:], in0=bank[:], scalar=1.0, in1=gb[:, cs], op0=ALU.add, op1=ALU.mult),
                       r=[bank, gb], w=[mt])
                else:
                    op("scalar", lambda e, mt=mt, bank=bank: e.copy(out=mt[:], in_=bank[:]), r=[bank], w=[mt])
                if which == 0:
                    idx = {0: 0, 1: 1, 2: 2, 3: 3, 4: 4, 5: 5}[j]
                else:
                    idx = {0: 6, 1: 7}[j]
                dma("sync", MODS[idx][:, cs], mt[:], r=[mt], key=mt)
    kb.end_phase()

    if "p1" in phases:
        kb.begin_phase()
        win = kb.sb("win", [128, 8, IN_WIDTH], BF16)
        wksw = kb.sb("wksw", [128, 8, 32], BF16)
        binT = kb.sb("binT", [128, 33], F32)
        A1 = kb.sb("A1", [128, D], F32)
        B1 = kb.sb("B1", [128, D], F32)
        Ac = kb.sb("Ac", [128, D], F32)
        Bc = kb.sb("Bc", [128, D], F32)
        qn2 = kb.sb("qn2", [128, 2], F32)
        kvn = kb.sb("kvn", [128, 1], F32)
        wuq = kb.sb("wuq", [128, 2, H * DQ], BF16)
        wuqsw = kb.sb("wuqsw", [128, 2, H * DQ], BF16)
        wkv = kb.sb("wkv", [128, 1024], BF16)
        hcw = kb.sb("hcw", [128, 36], F32)
        hcb = kb.sb("hcb", [128, 12], F32)
        ptall = kb.sb("ptall", [128, 12, 516], F32)
        win_v = win_d.rearrange("(k p) n -> p k n", p=128)
        for k in range(8):
            dma("gpsimd", win[:, k, :], win_v[:, k, :], w=[win], key=win)
        dma("gpsimd", wksw[:], wkpesw_d.rearrange("(k p) n -> p k n", p=128), w=[wksw], key=wksw)
        dma("sync", binT[:], binT_d, w=[binT], key=binT)
        for t_, i_ in ((A1, 1), (B1, 0), (Ac, 7), (Bc, 6)):
            dma("sync", t_[:], MODS[i_], w=[t_], key=t_)
        dma("sync", qn2[:], qnormT_d, w=[qn2], key=qn2)
        dma("sync", kvn[:], kvnormT_d, w=[kvn], key=kvn)
        dma("sync", hcw[:], hcw_d, w=[hcw], key=hcw)
        dma("sync", hcb[:], hcb_d, w=[hcb], key=hcb)
        op("vector", lambda e: e.memset(ptall[:], 0.0), w=[ptall])
        xtr = kb.rot("xt", 2, [128, D], F32)
        h32r = kb.rot("h32", 1, [128, D], F32)
        stg = h32r.bufs[0]
        for src_d, dst in ((wuq_d, wuq), (wuqsw_d, wuqsw)):
            for k in range(2):
                dma("sync", stg[:, 0:H * DQ], src_d[k * 128:(k + 1) * 128, :], w=[stg], key=stg)
                op("vector", lambda e, dst=dst, k=k: e.tensor_scalar(
                    out=dst[:, k, :], in0=stg[:, 0:H * DQ], scalar1=qn2[:, k:k + 1], scalar2=None, op0=ALU.mult),
                   r=[stg, qn2], w=[dst])
        dma("sync", stg[:, 0:512], wukvk_d, w=[stg], key=stg)
        dma("sync", stg[:, 512:1024], wukvv_d, w=[stg], key=stg)
        op("vector", lambda e: e.tensor_scalar(out=wkv[:], in0=stg[:], scalar1=kvn[:, 0:1], scalar2=None, op0=ALU.mult),
           r=[stg, kvn], w=[wkv])

        junkr = kb.rot("junk", 1, [128, D], BF16)
        str_ = kb.rot("st", 4, [128, 4], F32)
        hr = kb.rot("h", 2, [128, D], BF16)
        hTr = kb.rot("hT", 2, [128, 8, 512], BF16)
        qlr = kb.rot("ql", 2, [128, 3, 512], BF16)
        sqr = kb.rot("sq", 2, [128, 512], F32)
        rsr = kb.rot("rs", 1, [128, 2, 512], F32)
        rcolr = kb.rot("rcol", 2, [128, 4], F32)
        ropr = kb.rot("rop", 1, [96, 2, 512], F32)
        tmpr = kb.rot("tmp", 3, [96, 512], F32)
        qor = kb.rot("qo", 3, [96, 512], BF16)
        kor = kb.rot("ko", 3, [96, 512], BF16)
        vor = kb.rot("vo", 2, [128, 512], BF16)
        ubr = kb.rot("ub", 3, [128, 512], BF16)
        gbr = kb.rot("gb", 3, [128, 512], BF16)
        kpr = kb.rot("kp", 1, [32, 2, 512], F32)

        def proj_chunk(tok0, ntok, is_ctx):
            nt = ntok // 128
            src = ctx_d if is_ctx else x_d
            A, B = (Ac, Bc) if is_ctx else (A1, B1)
            key0 = tok0 if is_ctx else NCTX + tok0
            hT = hTr.next()
            for i in range(nt):
                xt = xtr.next()
                dma("sync", xt[:], src[tok0 + i * 128: tok0 + (i + 1) * 128, :], w=[xt], key=xt)
                junk = junkr.next()
                st = str_.next()
                op("scalar", lambda e, junk=junk, xt=xt, st=st: e.activation(
                    out=junk[:], in_=xt[:], func=AF.Square, accum_out=st[:, 0:1]), r=[xt], w=[junk, st])
                op("scalar", lambda e, st=st: e.activation(
                    out=st[:, 1:2], in_=st[:, 0:1], func=AF.Sqrt, scale=1.0 / D, bias=EPS), r=[st], w=[st])
                op("vector", lambda e, st=st: e.reciprocal(out=st[:, 2:3], in_=st[:, 1:2]), r=[st], w=[st])
                h32 = h32r.next()
                op("vector", lambda e, h32=h32, xt=xt, st=st, A=A: e.scalar_tensor_tensor(
                    out=h32[:], in0=xt[:], scalar=st[:, 2:3], in1=A[:], op0=ALU.mult, op1=ALU.mult),
                   r=[xt, st, A], w=[h32])
                hb = hr.next()
                op("gpsimd", lambda e, hb=hb, h32=h32, B=B: e.tensor_tensor(out=hb[:], in0=h32[:], in1=B[:], op=ALU.add),
                   r=[h32, B], w=[hb])
                for half in range(2):
                    bank = prot.next()
                    bankb = kb.Pb[:, bank_index(bank) * 1024: bank_index(bank) * 1024 + 512]
                    for kk in range(4):
                        k = half * 4 + kk
                        op("tensor", lambda e, bankb=bankb, kk=kk, hb=hb, k=k: e.transpose(
                            out=bankb[:, kk * 128:(kk + 1) * 128], in_=hb[:, k * 128:(k + 1) * 128], identity=identb[:]),
                           r=[hb, identb], w=[bank], inc=(kk == 3))
                    op("vector", lambda e, hT=hT, half=half, i=i, bankb=bankb: e.tensor_copy(
                        out=hT[:, half * 4:half * 4 + 4, i * 128:(i + 1) * 128],
                        in_=bankb.rearrange("p (k t) -> p k t", t=128)), r=[bank], w=[hT])

            yield

            def proj(c0, m, dst_bank, wsrc=None):
                for k in range(8):
                    lhs = win[:, k, c0:c0 + m] if wsrc is None else wsrc[:, k, :]
                    op("tensor", lambda e, lhs=lhs, k=k, dst_bank=dst_bank: e.matmul(
                        dst_bank[0:m, 0:ntok], lhsT=lhs, rhs=hT[:, k, 0:ntok], start=(k == 0), stop=(k == 7)),
                       r=[win if wsrc is None else wsrc, hT], w=[dst_bank], inc=(k == 7))

            ql = qlr.next()
            rs = rsr.next()
            rcol = rcolr.next()
            lat_tiles = ((2, C_KV, 1),) if is_ctx else ((0, C_Q, 0), (1, C_Q + 128, 0), (2, C_KV, 1))
            ssb = {0: prot.next(), 1: prot.next()}
            for (slot, c0, grp) in lat_tiles:
                bank = prot.next()
                proj(c0, 128, bank)
                bcol = {0: 0, 1: 1, 2: 2}[slot]
                op("scalar", lambda e, ql=ql, slot=slot, bank=bank, bcol=bcol: e.activation(
                    out=ql[:, slot, 0:ntok], in_=bank[:, 0:ntok], func=AF.Identity, bias=binT[:, bcol:bcol + 1]),
                   r=[bank, binT], w=[ql])
                sq = sqr.next()
                op("scalar", lambda e, sq=sq, bank=bank, bcol=bcol: e.activation(
                    out=sq[:, 0:ntok], in_=bank[:, 0:ntok], func=AF.Square, bias=binT[:, bcol:bcol + 1]),
                   r=[bank, binT], w=[sq])
                first = (grp == 1) or (slot == 0)
                last = (grp == 1) or (slot == 1)
                op("tensor", lambda e, sq=sq, grp=grp, first=first, last=last: e.matmul(
                    ssb[grp][:, 0:ntok], lhsT=ones[:], rhs=sq[:, 0:ntok], start=first, stop=last),
                   r=[ones, sq], w=[ssb[grp]], inc=last)
            for grp, width in ((0, 256.0), (1, 128.0)):
                if is_ctx and grp == 0:
                    continue
                op("scalar", lambda e, rs=rs, grp=grp, width=width: e.activation(
                    out=rs[:, grp, 0:ntok], in_=ssb[grp][:, 0:ntok], func=AF.Sqrt, scale=1.0 / width, bias=EPS),
                   r=[ssb[grp]], w=[rs])
                op("vector", lambda e, rs=rs, grp=grp: e.reciprocal(out=rs[:, grp, 0:ntok], in_=rs[:, grp, 0:ntok]),
                   r=[rs], w=[rs])
            if not is_ctx:
                op("vector", lambda e, rs=rs: e.tensor_scalar(
                    out=rs[:, 0, 0:ntok], in0=rs[:, 0, 0:ntok], scalar1=ATTN_SCALE, scalar2=None, op0=ALU.mult),
                   r=[rs], w=[rs])

            kp = kpr.next()
            bank = prot.next()
            proj(C_KPE, 32, bank)
            op("scalar", lambda e, kp=kp, bank=bank: e.activation(
                out=kp[:, 0, 0:ntok], in_=bank[0:32, 0:ntok], func=AF.Identity, bias=binT[0:32, 31:32]),
               r=[bank, binT], w=[kp])
            if not is_ctx:
                rop = ropr.next()
                dma("sync", rop[64:96, 0, :], ropeC_d[:, tok0:tok0 + 512], w=[rop], key=rop)
                dma("sync", rop[64:96, 1, :], ropeS_d[:, tok0:tok0 + 512], w=[rop], key=rop)
                dma("sync", rop[0:32, 0, :], ropeC_d[:, tok0:tok0 + 512], w=[rop], key=rop)
                dma("sync", rop[0:32, 1, :], ropeS_d[:, tok0:tok0 + 512], w=[rop], key=rop)
                bank2 = prot.next()
                proj(0, 32, bank2, wsrc=wksw)
                op("scalar", lambda e, kp=kp, bank2=bank2: e.activation(
                    out=kp[:, 1, 0:ntok], in_=bank2[0:32, 0:ntok], func=AF.Identity, bias=binT[0:32, 32:33]),
                   r=[bank2, binT], w=[kp])
                op("vector", lambda e, kp=kp, rop=rop: e.tensor_tensor(
                    out=kp[:, 0, :], in0=kp[:, 0, :], in1=rop[0:32, 0, :], op=ALU.mult), r=[kp, rop], w=[kp])
                op("vector", lambda e, kp=kp, rop=rop: e.tensor_tensor(
                    out=kp[:, 1, :], in0=kp[:, 1, :], in1=rop[0:32, 1, :], op=ALU.mult), r=[kp, rop], w=[kp])
                op("vector", lambda e, kp=kp: e.tensor_tensor(
                    out=kp[:, 0, :], in0=kp[:, 0, :], in1=kp[:, 1, :], op=ALU.add), r=[kp], w=[kp])
                op("vector", lambda e, rop=rop, rs=rs: e.tensor_tensor(
                    out=rop[64:96, :, :], in0=rop[64:96, :, :],
                    in1=rs[64:96, 0:1, :].to_broadcast([32, 2, 512]), op=ALU.mult), r=[rop, rs], w=[rop])

            for hd in range(H):
                ko = kor.next()
                bank = prot.next()
                op("tensor", lambda e, bank=bank, hd=hd, ql=ql: e.matmul(
                    bank[0:64, 0:ntok], lhsT=wkv[:, hd * 64:(hd + 1) * 64], rhs=ql[:, 2, 0:ntok], start=True, stop=True),
                   r=[wkv, ql], w=[bank])
                op("vector", lambda e, ko=ko, bank=bank, rs=rs: e.tensor_tensor(
                    out=ko[0:64, 0:ntok], in0=bank[0:64, 0:ntok], in1=rs[0:64, 1, 0:ntok], op=ALU.mult),
                   r=[bank, rs], w=[ko])
                dma("sync", Ks[hd, 0:64, key0:key0 + ntok], ko[0:64, 0:ntok], r=[ko], key=ko)
            kpbf = qor.next()
            op("scalar", lambda e, kpbf=kpbf, kp=kp: e.copy(out=kpbf[0:32, 0:ntok], in_=kp[:, 0, 0:ntok]), r=[kp], w=[kpbf])
            for hd in range(H):
                dma("sync", Ks[hd, 64:96, key0:key0 + ntok], kpbf[0:32, 0:ntok], r=[kpbf], key=kpbf)

            if not is_ctx:
                for hd in range(H):
                    qo = qor.next()
                    bank = prot.next()
                    bank2 = prot.next()
                    for k in range(2):
                        op("tensor", lambda e, bank=bank, hd=hd, k=k, ql=ql: e.matmul(
                            bank[0:96, :], lhsT=wuq[:, k, hd * DQ:(hd + 1) * DQ], rhs=ql[:, k, :], start=(k == 0), stop=(k == 1)),
                           r=[wuq, ql], w=[bank], inc=(k == 1))
                    for k in range(2):
                        op("tensor", lambda e, bank2=bank2, hd=hd, k=k, ql=ql: e.matmul(
                            bank2[0:96, :], lhsT=wuqsw[:, k, hd * DQ:(hd + 1) * DQ], rhs=ql[:, k, :], start=(k == 0), stop=(k == 1)),
                           r=[wuqsw, ql], w=[bank2], inc=(k == 1))
                    op("vector", lambda e, qo=qo, bank=bank, rs=rs: e.tensor_tensor(
                        out=qo[0:64, :], in0=bank[0:64, :], in1=rs[0:64, 0, :], op=ALU.mult), r=[bank, rs], w=[qo])
                    tmp = tmpr.next()
                    op("vector", lambda e, tmp=tmp, bank=bank, rop=rop: e.tensor_tensor(
                        out=tmp[64:96, :], in0=bank[64:96, :], in1=rop[64:96, 0, :], op=ALU.mult), r=[bank, rop], w=[tmp])
                    tmp2 = tmpr.next()
                    op("vector", lambda e, tmp2=tmp2, bank2=bank2, rop=rop: e.tensor_tensor(
                        out=tmp2[64:96, :], in0=bank2[64:96, :], in1=rop[64:96, 1, :], op=ALU.mult), r=[bank2, rop], w=[tmp2])
                    op("gpsimd", lambda e, qo=qo, tmp=tmp, tmp2=tmp2: e.tensor_tensor(
                        out=qo[64:96, :], in0=tmp[64:96, :], in1=tmp2[64:96, :], op=ALU.add), r=[tmp, tmp2], w=[qo])
                    dma("sync", Qs[hd, :, tok0:tok0 + 512], qo[:, :], r=[qo], key=qo)

            for i in range(nt):
                bank = prot.next()
                op("tensor", lambda e, bank=bank, i=i, ql=ql: e.matmul(
                    bank[:, :], lhsT=ql[:, 2, i * 128:(i + 1) * 128], rhs=wkv[:, 512:1024], start=True, stop=True),
                   r=[ql, wkv], w=[bank])
                bank3 = prot.next()
                op("tensor", lambda e, bank3=bank3, i=i, rs=rs: e.transpose(
                    out=bank3[:, 0:128], in_=rs[:, 1, i * 128:(i + 1) * 128], identity=ident[:]),
                   r=[rs, ident], w=[bank3])
                op("scalar", lambda e, rcol=rcol, bank3=bank3, i=i: e.copy(out=rcol[:, i:i + 1], in_=bank3[:, 0:1]),
                   r=[bank3], w=[rcol])
                vo = vor.next()
                op("vector", lambda e, vo=vo, bank=bank, rcol=rcol, i=i: e.tensor_scalar(
                    out=vo[:], in0=bank[:], scalar1=rcol[:, i:i + 1], scalar2=None, op0=ALU.mult), r=[bank, rcol], w=[vo])
                dma("sync", Vs[key0 + i * 128: key0 + (i + 1) * 128, :], vo[:], r=[vo], key=vo)

            if is_ctx:
                return
            for j in range(12):
                bank = prot.next()
                proj(C_HY + j * 128, 128, bank)
                op("scalar", lambda e, j=j: e.copy(out=ptall[:, j, 0:2], in_=ptall[:, j, 512:514]), r=[ptall], w=[ptall])
                op("scalar", lambda e, j=j, bank=bank: e.activation(
                    out=ptall[:, j, 2:514], in_=bank[:, :], func=AF.Identity, bias=binT[:, 3 + j:4 + j]),
                   r=[bank, binT], w=[ptall])
                ub = ubr.next()
                conv_tile(ub, j)
                dma("sync", Us[j * 128:(j + 1) * 128, tok0:tok0 + 512], ub[:], r=[ub], key=ub)
            for j in range(16):
                bank = prot.next()
                proj(C_GATE + j * 128, 128, bank)
                gb = gbr.next()
                op("scalar", lambda e, gb=gb, bank=bank, j=j: e.activation(
                    out=gb[:], in_=bank[:], func=AF.Sigmoid, bias=binT[:, 15 + j:16 + j]), r=[bank, binT], w=[gb])
                dma("sync", Gs[j * 128:(j + 1) * 128, tok0:tok0 + 512], gb[:], r=[gb], key=gb)

        cvr = kb.rot("cv", 2, [128, 512], F32)

        def conv_tile(ub, j, n=512, src0=0):
            cv = cvr.next()
            op("vector", lambda e, cv=cv, j=j: e.tensor_scalar(
                out=cv[:, 0:n], in0=ptall[:, j, src0:src0 + n], scalar1=hcw[:, 3 * j:3 * j + 1], scalar2=hcb[:, j:j + 1],
                op0=ALU.mult, op1=ALU.add), r=[ptall, hcw, hcb], w=[cv])
            op("vector", lambda e, cv=cv, j=j: e.scalar_tensor_tensor(
                out=cv[:, 0:n], in0=ptall[:, j, src0 + 1:src0 + 1 + n], scalar=hcw[:, 3 * j + 1:3 * j + 2], in1=cv[:, 0:n],
                op0=ALU.mult, op1=ALU.add), r=[ptall, hcw, cv], w=[cv])
            op("vector", lambda e, cv=cv, j=j, ub=ub: e.scalar_tensor_tensor(
                out=ub[:, 0:n], in0=ptall[:, j, src0 + 2:src0 + 2 + n], scalar=hcw[:, 3 * j + 2:3 * j + 3], in1=cv[:, 0:n],
                op0=ALU.mult, op1=ALU.add), r=[ptall, hcw, cv], w=[ub])

        def bank_index(bank):
            return pb.index(bank)

        nchunks = SEQ // 512
        cgens = [proj_chunk(0, 256, True)] + [proj_chunk(ci * 512, 512, False) for ci in range(nchunks)]
        next(cgens[0])
        for ci_ in range(len(cgens)):
            if ci_ + 1 < len(cgens):
                next(cgens[ci_ + 1])
            for _ in cgens[ci_]:
                pass
        for j in range(12):
            op("scalar", lambda e, j=j: e.copy(out=ptall[:, j, 0:2], in_=ptall[:, j, 512:514]), r=[ptall], w=[ptall])
            op("vector", lambda e, j=j: e.memset(ptall[:, j, 2:3], 0.0), w=[ptall])
            ub = ubr.next()
            conv_tile(ub, j, n=1, src0=0)
            dma("sync", Us[j * 128:(j + 1) * 128, SEQ:SEQ + 1], ub[:, 0:1], r=[ub], key=ub, allow_slow_non_contiguous=True)
        kb.end_phase()


    EWg = kb.scratch("EWg", [NE, 128, 8 * EFF], BF16)
    EWu = kb.scratch("EWu", [NE, 128, 8 * EFF], BF16)
    EWd = kb.scratch("EWd", [NE, 128, 2 * D], BF16)
    ne_lim = lim.get("experts", NE)
    if "moe" in phases:
        weg_d = kb.inp("w_exp_gate", [NE, D, EFF]); weu_d = kb.inp("w_exp_up", [NE, D, EFF]); wed_d = kb.inp("w_exp_down", [NE, EFF, D])

    def convert_expert_weights():
        cvb = [Buf("cv%d" % i) for i in range(4)]
        kb.phase_bufs.extend(cvb)
        for e_ in range(ne_lim):
            dma("gpsimd", EWg[e_].rearrange("p (k f) -> p k f", k=8), weg_d[e_].rearrange("(k p) f -> p k f", p=128), key=cvb[e_ % 4])
            dma("gpsimd", EWu[e_].rearrange("p (k f) -> p k f", k=8), weu_d[e_].rearrange("(k p) f -> p k f", p=128), key=cvb[e_ % 4])
            dma("gpsimd", EWd[e_].rearrange("p (k d) -> p k d", k=2), wed_d[e_].rearrange("(k p) d -> p k d", p=128), key=cvb[e_ % 4])
    converted = [False]

    ATs = kb.scratch("ATs", [512, SEQ], BF16)
    if "attn" in phases:
        kb.begin_phase()
        Ktr = kb.rot("Kt", 2, [96, NKEY], BF16)
        Vtr = kb.rot("Vt", 2, [128, 66, 128], BF16)
        Qtr = kb.rot("Qt", 2, [96, SEQ], BF16)
        ptr_ = kb.rot("pt", 4, [128, 1024], BF16)
        osr = kb.rot("os", 2, [64, 512], F32)
        rdr = kb.rot("rd", 2, [128, 512], F32)
        aor = kb.rot("ao", 2, [64, 512], BF16)
        for vt in Vtr.bufs:
            op("vector", lambda e, vt=vt: e.memset(vt[:, :, 64:128], 1.0), w=[vt])
        if "moe" in phases:
            convert_expert_weights()
            converted[0] = True
        Vs_v = Vs.rearrange("(kt p) c -> p kt c", p=128)

        def load_head(hd):
            kt_, vt_, qt_ = Ktr.next(), Vtr.next(), Qtr.next()
            dma("sync", kt_[:], Ks[hd], w=[kt_], key=kt_)
            dma("sync", vt_[:, :, 0:64], Vs_v[:, :, hd * 64:(hd + 1) * 64], w=[vt_], key=vt_)
            dma("sync", qt_[:], Qs[hd], w=[qt_], key=qt_)
            return kt_, vt_, qt_

        spair = [(pb[0], pb[1]), (pb[2], pb[3]), (pb[4], pb[5])]
        pobanks = [pb[6], pb[6]]
        bcbanks = [pb[7], pb[7]]
        items = []
        for hd in range(H):
            for qc in range(SEQ // 512):
                for pi in range(33):
                    items.append((hd, qc, pi))
        heads = {0: load_head(0)}
        state = {}
        pend = []

        def emit_S(i):
            hd, qc, pi = items[i]
            if qc == 1 and pi == 0 and hd + 1 < H:
                heads[hd + 1] = load_head(hd + 1)
            kt_, vt_, qt_ = heads[hd]
            b0, b1 = spair[i % 3]
            for j, bk in enumerate((b0, b1)):
                ktile = pi * 2 + j
                op("tensor", lambda e, bk=bk, kt_=kt_, qt_=qt_, ktile=ktile, qc=qc: e.matmul(
                    bk[:, :], lhsT=kt_[:, ktile * 128:(ktile + 1) * 128], rhs=qt_[:, qc * 512:(qc + 1) * 512],
                    start=True, stop=True), r=[kt_, qt_], w=[bk])
            pt = ptr_.next()
            state[i] = pt
            bi = pb.index(b0)
            op("scalar", lambda e, pt=pt, bi=bi: e.activation(out=pt[:, :], in_=kb.P[:, bi * 512:bi * 512 + 1024], func=AF.Exp),
               r=[b0, b1], w=[pt])

        def emit_PV(i):
            hd, qc, pi = items[i]
            kt_, vt_, qt_ = heads[hd]
            pt = state.pop(i)
            g = hd * (SEQ // 512) + qc
            po = pobanks[g % 2]
            for j in range(2):
                ktile = pi * 2 + j
                op("tensor", lambda e, po=po, vt_=vt_, pt=pt, ktile=ktile, j=j: e.matmul(
                    po[:, :], lhsT=vt_[:, ktile, :], rhs=pt[:, j * 512:(j + 1) * 512],
                    start=(ktile == 0), stop=(ktile == 65)), r=[vt_, pt], w=[po], inc=(j == 1))
            if pi == 32:
                osb, rd, bc = osr.next(), rdr.next(), bcbanks[g % 2]
                op("scalar", lambda e, osb=osb, po=po: e.copy(out=osb[:, :], in_=po[0:64, :]), r=[po], w=[osb])
                op("vector", lambda e, rd=rd, po=po: e.reciprocal(out=rd[64:65, :], in_=po[64:65, :]), r=[po], w=[rd])

                def fin(hd=hd, qc=qc, osb=osb, rd=rd, bc=bc):
                    op("tensor", lambda e: e.matmul(bc[0:64, :], lhsT=ones[64:65, 0:64], rhs=rd[64:65, :], start=True, stop=True),
                       r=[ones, rd], w=[bc])
                    ao = aor.next()
                    op("vector", lambda e, ao=ao: e.tensor_tensor(out=ao[:, :], in0=osb[:, :], in1=bc[0:64, :], op=ALU.mult),
                       r=[osb, bc], w=[ao])
                    dma("sync", ATs[hd * 64:(hd + 1) * 64, qc * 512:(qc + 1) * 512], ao[:, :], r=[ao], key=ao)
                pend.append((i + 3, fin))

        n = len(items)
        DEPTH = 2
        for i in range(n + DEPTH):
            if i < n:
                emit_S(i)
            if i >= DEPTH:
                emit_PV(i - DEPTH)
            while pend and pend[0][0] <= i:
                pend.pop(0)[1]()
        while pend:
            pend.pop(0)[1]()
        kb.end_phase()


    FTs = kb.scratch("FTs", [2048, SEQ], BF16)
    KFs = kb.scratch("KFs", [2, 128, 2, HYW, 128], BF16)
    HYs = kb.scratch("HYs", [64, HYW, 128], BF16)
    need_fft = ("filt" in phases) or ("hyena" in phases)
    if need_fft:
        dC_d = kb.inp("dftC", [128, 128]); dS_d = kb.inp("dftS", [128, 128])
        twC_d = kb.inp("twC", [128, 128]); twS_d = kb.inp("twS", [128, 128])
        tC = kb.sb("tC", [128, 128], BF16, persist=True)
        tS = kb.sb("tS", [128, 128], BF16, persist=True)
        tnS = kb.sb("tnS", [128, 128], BF16, persist=True)
        tCS = kb.sb("tCS", [128, 256], BF16, persist=True)
        tSnC = kb.sb("tSnC", [128, 256], BF16, persist=True)
        tCShi = kb.sb("tCShi", [64, 256], BF16, persist=True)
        twC = kb.sb("twC", [128, 128], F32, persist=True)
        twS = kb.sb("twS", [128, 128], F32, persist=True)
        kb.begin_phase()
        stgC = kb.sb("stgC", [128, 128], F32); stgS = kb.sb("stgS", [128, 128], F32)
        stgH = kb.sb("stgH", [64, 256], F32)
        dma("sync", stgC[:], dC_d, w=[stgC], key=stgC)
        dma("sync", stgS[:], dS_d, w=[stgS], key=stgS)
        dma("sync", stgH[:, 0:128], dC_d[64:128, :], w=[stgH], key=stgH)
        dma("sync", stgH[:, 128:256], dS_d[64:128, :], w=[stgH], key=stgH)
        dma("sync", twC[:], twC_d, w=[twC], key=twC)
        dma("sync", twS[:], twS_d, w=[twS], key=twS)
        op("vector", lambda e: e.tensor_copy(out=tC[:], in_=stgC[:]), r=[stgC], w=[tC])
        op("vector", lambda e: e.tensor_copy(out=tS[:], in_=stgS[:]), r=[stgS], w=[tS])
        op("vector", lambda e: e.tensor_scalar(out=tnS[:], in0=stgS[:], scalar1=-1.0, scalar2=None, op0=ALU.mult), r=[stgS], w=[tnS])
        op("vector", lambda e: e.tensor_copy(out=tCS[:, 0:128], in_=stgC[:]), r=[stgC], w=[tCS])
        op("vector", lambda e: e.tensor_copy(out=tCS[:, 128:256], in_=stgS[:]), r=[stgS], w=[tCS])
        op("vector", lambda e: e.tensor_copy(out=tSnC[:, 0:128], in_=stgS[:]), r=[stgS], w=[tSnC])
        op("vector", lambda e: e.tensor_scalar(out=tSnC[:, 128:256], in0=stgC[:], scalar1=-1.0, scalar2=None, op0=ALU.mult), r=[stgC], w=[tSnC])
        op("vector", lambda e: e.tensor_copy(out=tCShi[:], in_=stgH[:]), r=[stgH], w=[tCShi])
        kb.end_phase()

    pairs = Rot([(pb[0], pb[1]), (pb[2], pb[3]), (pb[4], pb[5]), (pb[6], pb[7])])

    def pair_ap(pr):
        i0 = pb.index(pr[0])
        return kb.P[:, i0 * 512:i0 * 512 + 1024]

    def interleave(gens, width):
        gens = iter(gens)
        active = []
        done = False
        while True:
            while not done and len(active) < width:
                try:
                    active.append(next(gens))
                except StopIteration:
                    done = True
            if not active:
                break
            for g_ in list(active):
                try:
                    next(g_)
                except StopIteration:
                    active.remove(g_)

    class FFT:
        def __init__(self):
            self.tar = kb.rot("ta", 3, [128, 1024], F32)
            self.tbr = kb.rot("tb", 3, [128, 1024], F32)
            self.brr = kb.rot("br", 6, [128, 4, 128], BF16)
            self.bir = kb.rot("bi", 6, [128, 4, 128], BF16)

        def cmul(self, pr, lay, tcv, tsv, tabs, o_re, o_im, obufs_re, obufs_im):
            pa = pair_ap(pr)
            ta, tb = self.tar.next(), self.tbr.next()
            if lay == 1:
                pv = pa.rearrange("p (c t k) -> p c t k", c=4, t=2)
                tav = ta[:].rearrange("p (c t k) -> p c t k", c=4, t=2)
                tbv = tb[:].rearrange("p (c t k) -> p c t k", c=4, t=2)
                a0, a1 = tav[:, :, 0, :], tav[:, :, 1, :]
                b0, b1 = tbv[:, :, 0, :], tbv[:, :, 1, :]
            else:
                pv = pa.rearrange("p (t c k) -> p t c k", t=2, c=4)
                tav = ta[:].rearrange("p (t c k) -> p t c k", t=2, c=4)
                tbv = tb[:].rearrange("p (t c k) -> p t c k", t=2, c=4)
                a0, a1 = tav[:, 0], tav[:, 1]
                b0, b1 = tbv[:, 0], tbv[:, 1]
            op("vector", lambda e: e.tensor_tensor(out=tav, in0=pv, in1=tcv, op=ALU.mult), r=[pr[0], pr[1]] + tabs, w=[ta])
            op("vector", lambda e: e.tensor_tensor(out=tbv, in0=pv, in1=tsv, op=ALU.mult), r=[pr[0], pr[1]] + tabs, w=[tb])
            op("gpsimd", lambda e: e.tensor_tensor(out=o_re, in0=a0, in1=b1, op=ALU.subtract), r=[ta, tb], w=obufs_re)
            op("gpsimd", lambda e: e.tensor_tensor(out=o_im, in0=b0, in1=a1, op=ALU.add), r=[ta, tb], w=obufs_im)

        def forward(self, srcs):
            pr = pairs.next()
            pa = pair_ap(pr)
            for c in range(4):
                for si, (sbuf, sap, tbuf, tap) in enumerate(srcs):
                    last = si == len(srcs) - 1
                    op("tensor", lambda e, c=c, sap=sap, tap=tap, si=si, last=last: e.matmul(
                        pa[:, c * 256:(c + 1) * 256], lhsT=sap[:, c, :], rhs=tap, start=(si == 0), stop=last),
                       r=[sbuf, tbuf], w=[pr[0], pr[1]], inc=(last and c == 3))
            yield
            br, bi = self.brr.next(), self.bir.next()
            tcv = twC[:].unsqueeze(1).unsqueeze(1).to_broadcast([128, 4, 2, 128])
            tsv = twS[:].unsqueeze(1).unsqueeze(1).to_broadcast([128, 4, 2, 128])
            self.cmul(pr, 1, tcv, tsv, [twC, twS], br[:], bi[:], [br], [bi])
            yield
            pr2 = pairs.next()
            brv = br[:].rearrange("p c k -> p (c k)")
            biv = bi[:].rearrange("p c k -> p (c k)")
            for (bank, la, lb) in ((pr2[0], tC, tnS), (pr2[1], tS, tC)):
                op("tensor", lambda e, bank=bank, la=la: e.matmul(bank[:, :], lhsT=la[:], rhs=brv, start=True, stop=False),
                   r=[la, br], w=[bank], inc=False)
                op("tensor", lambda e, bank=bank, lb=lb: e.matmul(bank[:, :], lhsT=lb[:], rhs=biv, start=False, stop=True),
                   r=[lb, bi], w=[bank])
            yield
            return pr2

        def inverse(self, yre, yim):
            pr = pairs.next()
            pa = pair_ap(pr)
            for c in range(4):
                op("tensor", lambda e, c=c: e.matmul(pa[:, c * 256:(c + 1) * 256], lhsT=yre[:, c, :], rhs=tCS[:], start=True, stop=False),
                   r=[yre, tCS], w=[pr[0], pr[1]], inc=False)
                op("tensor", lambda e, c=c: e.matmul(pa[:, c * 256:(c + 1) * 256], lhsT=yim[:, c, :], rhs=tSnC[:], start=False, stop=True),
                   r=[yim, tSnC], w=[pr[0], pr[1]], inc=(c == 3))
            yield
            hr_, hi_ = self.brr.next(), self.bir.next()
            tcv = twC[:].unsqueeze(1).unsqueeze(1).to_broadcast([128, 4, 2, 128])
            tsv = twS[:].unsqueeze(1).unsqueeze(1).to_broadcast([128, 4, 2, 128])
            self.cmul(pr, 1, tcv, tsv, [twC, twS], hr_[:], hi_[:], [hr_], [hi_])
            yield
            pr3 = pairs.next()
            bank = pr3[0]
            op("tensor", lambda e: e.matmul(bank[0:64, :], lhsT=tC[:, 0:64], rhs=hr_[:].rearrange("p c k -> p (c k)"), start=True, stop=False),
               r=[tC, hr_], w=[bank], inc=False)
            op("tensor", lambda e: e.matmul(bank[0:64, :], lhsT=tnS[:, 0:64], rhs=hi_[:].rearrange("p c k -> p (c k)"), start=False, stop=True),
               r=[tnS, hi_], w=[bank])
            yield
            return bank

    if "filt" in phases:
        embF_d = kb.inp("embF", [17, SEQ]); embB_d = kb.inp("embB", [17, SEQ])
        w1_d = kb.inp("hf_w1", [17, 64]); w2_d = kb.inp("hf_w2", [64, 64]); w3_d = kb.inp("hf_w3", [64, 2048])
        hfv_d = kb.inp("hf_vecs", [64, 3])
        trF_d = kb.inp("trowF", [128, SEQ]); trB_d = kb.inp("trowB", [128, SEQ])
        ndl_d = kb.inp("negdelta", [128, 4])
        skb_d = kb.inp("skip_b", [128, 2, HYW])
        kb.begin_phase()
        w1 = kb.sb("w1", [17, 64], F32); w2 = kb.sb("w2", [64, 64], F32); w3 = kb.sb("w3", [64, 2048], F32)
        hfv = kb.sb("hfv", [64, 8], F32)
        ndl = kb.sb("ndl", [128, 4], F32)
        h2T = [kb.sb("h2TF", [64, SEQ], F32), kb.sb("h2TB", [64, SEQ], F32)]
        embr = kb.rot("emb", 2, [17, 512], F32)
        ur = kb.rot("u", 2, [64, 512], F32)
        u2r = kb.rot("u2", 2, [64, 512], F32)
        h1r = kb.rot("h1", 2, [64, 512], F32)
        l1p = kb.sb("l1p", [128, 16, 16], F32)
        dma("sync", w1[:], w1_d, w=[w1], key=w1)
        dma("sync", w2[:], w2_d, w=[w2], key=w2)
        dma("sync", w3[:], w3_d, w=[w3], key=w3)
        dma("sync", hfv[:, 0:3], hfv_d, w=[hfv], key=hfv)
        dma("sync", ndl[:], ndl_d, w=[ndl], key=ndl)
        TWO_PI = 2.0 * math.pi
        op("vector", lambda e: e.tensor_scalar(out=hfv[:, 3:4], in0=hfv[:, 2:3], scalar1=1.0 / TWO_PI, scalar2=None, op0=ALU.mult), r=[hfv], w=[hfv])
        op("vector", lambda e: e.tensor_tensor(out=hfv[:, 4:5], in0=hfv[:, 3:4], in1=hfv[:, 0:1], op=ALU.mult), r=[hfv], w=[hfv])
        op("vector", lambda e: e.tensor_tensor(out=hfv[:, 5:6], in0=hfv[:, 3:4], in1=hfv[:, 1:2], op=ALU.mult), r=[hfv], w=[hfv])

        def sin_layer(bank, bcol, dst, dbuf):
            u = ur.next()
            op("scalar", lambda e, u=u: e.activation(out=u[:], in_=bank[0:64, :], func=AF.Identity, scale=hfv[:, 3:4], bias=hfv[:, bcol:bcol + 1]),
               r=[bank, hfv], w=[u])
            u2 = u2r.next()
            for it in range(2):
                op("vector", lambda e, u=u, u2=u2: e.scalar_tensor_tensor(out=u2[:], in0=u[:], scalar=0.5, in1=u[:], op0=ALU.is_gt, op1=ALU.subtract),
                   r=[u], w=[u2])
                op("vector", lambda e, u=u, u2=u2: e.scalar_tensor_tensor(out=u[:], in0=u2[:], scalar=0.5, in1=u2[:], op0=ALU.is_gt, op1=ALU.subtract),
                   r=[u2], w=[u])
            op("scalar", lambda e, u=u: e.activation(out=dst, in_=u[:], func=AF.Sin, scale=6.283185), r=[u], w=[dbuf])

        for di, emb_d in enumerate((embF_d, embB_d)):
            for pc in range(16):
                em = embr.next()
                dma("sync", em[:], emb_d[:, pc * 512:(pc + 1) * 512], w=[em], key=em)
                bank = prot.next()
                op("tensor", lambda e, bank=bank, em=em: e.matmul(bank[0:64, :], lhsT=w1[:], rhs=em[:], start=True, stop=True), r=[w1, em], w=[bank])
                h1 = h1r.next()
                sin_layer(bank, 4, h1[:], h1)
                bank2 = prot.next()
                op("tensor", lambda e, bank2=bank2, h1=h1: e.matmul(bank2[0:64, :], lhsT=w2[:], rhs=h1[:], start=True, stop=True), r=[w2, h1], w=[bank2])
                sin_layer(bank2, 5, h2T[di][:, pc * 512:(pc + 1) * 512], h2T[di])

        trr = kb.rot("tr", 2, [128, 512], F32)
        winr = kb.rot("win", 2, [128, 512], F32)
        kwr = kb.rot("kw", 2, [128, 512], F32)
        kbr = kb.rot("kbf", 3, [128, 512], BF16)
        op("vector", lambda e: e.memset(l1p[:], 0.0), w=[l1p])
        for di, tr_d in enumerate((trF_d, trB_d)):
            for pc in range(16):
                tr = trr.next()
                dma("sync", tr[:], tr_d[:, pc * 512:(pc + 1) * 512], w=[tr], key=tr)
                for cq in range(4):
                    win = winr.next()
                    op("scalar", lambda e, win=win, tr=tr, cq=cq: e.activation(out=win[:], in_=tr[:], func=AF.Exp, scale=ndl[:, cq:cq + 1]),
                       r=[tr, ndl], w=[win])
                    for o in range(2):
                        ct = (2 * o + di) * 4 + cq
                        bank = prot.next()
                        op("tensor", lambda e, bank=bank, ct=ct, di=di, pc=pc: e.matmul(
                            bank[:, :], lhsT=w3[:, ct * 128:(ct + 1) * 128], rhs=h2T[di][:, pc * 512:(pc + 1) * 512], start=True, stop=True),
                           r=[w3, h2T[di]], w=[bank])
                        kw = kwr.next()
                        op("vector", lambda e, kw=kw, bank=bank, win=win: e.tensor_tensor(out=kw[:], in0=bank[:, :], in1=win[:], op=ALU.mult),
                           r=[bank, win], w=[kw])
                        op("vector", lambda e, kw=kw, ct=ct, pc=pc: e.tensor_reduce(
                            out=l1p[:, ct, pc:pc + 1], in_=kw[:], axis=AX.X, op=ALU.add, apply_absolute_value=True), r=[kw], w=[l1p])
                        kbf = kbr.next()
                        op("gpsimd", lambda e, kbf=kbf, kw=kw: e.tensor_copy(out=kbf[:], in_=kw[:]), r=[kw], w=[kbf])
                        dma("sync", FTs[ct * 128:(ct + 1) * 128, pc * 512:(pc + 1) * 512], kbf[:], r=[kbf], key=kbf)
        l1c = kb.sb("l1c", [128, 16], F32)
        rnc = kb.sb("rnc", [128, 8], F32)
        rnb = kb.sb("rnb", [128, 2, HYW], F32)
        skb = kb.sb("skb", [128, 2, HYW], F32)
        dg = kb.rot("dg", 2, [128, 128], F32)
        dma("sync", skb[:], skb_d, w=[skb], key=skb)
        op("vector", lambda e: e.tensor_scalar(out=skb[:], in0=skb[:], scalar1=1.0 / NFFT, scalar2=None, op0=ALU.mult), r=[skb], w=[skb])
        op("vector", lambda e: e.tensor_reduce(out=l1c[:], in_=l1p[:], axis=AX.X, op=ALU.add), r=[l1p], w=[l1c])
        for o in range(2):
            op("vector", lambda e, o=o: e.tensor_tensor(out=rnc[:, o * 4:(o + 1) * 4], in0=l1c[:, (2 * o) * 4:(2 * o) * 4 + 4],
                                                         in1=l1c[:, (2 * o + 1) * 4:(2 * o + 1) * 4 + 4], op=ALU.add), r=[l1c], w=[rnc])
        op("vector", lambda e: e.tensor_scalar(out=rnc[:], in0=rnc[:], scalar1=1e-6, scalar2=float(NFFT), op0=ALU.add, op1=ALU.mult), r=[rnc], w=[rnc])
        op("vector", lambda e: e.reciprocal(out=rnc[:], in_=rnc[:]), r=[rnc], w=[rnc])
        for o in range(2):
            for cq in range(4):
                d_ = dg.next()
                op("vector", lambda e, d_=d_, o=o, cq=cq: e.tensor_scalar(out=d_[:], in0=ident[:], scalar1=rnc[:, o * 4 + cq:o * 4 + cq + 1],
                                                                         scalar2=None, op0=ALU.mult), r=[ident, rnc], w=[d_])
                bank = prot.next()
                op("tensor", lambda e, bank=bank, d_=d_: e.matmul(bank[:, 0:128], lhsT=ones[:], rhs=d_[:], start=True, stop=True), r=[ones, d_], w=[bank])
                op("scalar", lambda e, bank=bank, o=o, cq=cq: e.copy(out=rnb[:, o, cq * 128:(cq + 1) * 128], in_=bank[:, 0:128]), r=[bank], w=[rnb])
        RNs = kb.scratch("RNs", [2, 128, 2 * HYW], F32)
        dma("sync", RNs[0], rnb[:].rearrange("p o c -> p (o c)"), r=[rnb], key=rnb)
        dma("sync", RNs[1], skb[:].rearrange("p o c -> p (o c)"), r=[skb], key=skb)
        kb.end_phase()
        kb.begin_phase()
        rnb = kb.sb("rnb2", [128, 2, HYW], F32)
        skb = kb.sb("skb2", [128, 2, HYW], F32)
        dma("sync", rnb[:].rearrange("p o c -> p (o c)"), RNs[0], w=[rnb], key=rnb)
        dma("sync", skb[:].rearrange("p o c -> p (o c)"), RNs[1], w=[skb], key=skb)
        fft = FFT()
        ffr = kb.rot("ff", 3, [64, 16, 128], BF16)
        fbr = kb.rot("fb", 3, [64, 16, 128], BF16)
        t32r = kb.rot("t32", 3, [128, 2, 4, 128], F32)
        kfr = kb.rot("kf", 2, [128, 2, 16, 128], BF16)
        def filt_block(o, cb):
            ff, fb = ffr.next(), fbr.next()
            r0 = (2 * o) * 512 + cb * 16
            r1 = (2 * o + 1) * 512 + cb * 16
            dma("sync", ff[:], FTs[r0:r0 + 16, :].rearrange("c (a b) -> a c b", b=128), w=[ff], key=ff)
            dma("sync", fb[:], FTs[r1:r1 + 16, :].rearrange("c (a b) -> a c b", b=128), w=[fb], key=fb)
            kf = kfr.next()

            def chain(g):
                pr2 = yield from fft.forward([(ff, ff[:, g * 4:(g + 1) * 4, :], tCS, tCS[0:64, :]),
                                              (fb, fb[:, g * 4:(g + 1) * 4, :], tCShi, tCShi[0:64, :])])
                c0 = cb * 16 + g * 4
                t32 = t32r.next()
                pv = pair_ap(pr2).rearrange("p (t c k) -> p t c k", t=2, c=4)
                op("vector", lambda e: e.tensor_tensor(
                    out=t32[:], in0=pv, in1=rnb[:, o, c0:c0 + 4].unsqueeze(1).unsqueeze(3).to_broadcast([128, 2, 4, 128]), op=ALU.mult),
                   r=[pr2[0], pr2[1], rnb], w=[t32])
                op("gpsimd", lambda e: e.tensor_tensor(
                    out=kf[:, 0, g * 4:(g + 1) * 4, :], in0=t32[:, 0], in1=skb[:, o, c0:c0 + 4].unsqueeze(2).to_broadcast([128, 4, 128]), op=ALU.add),
                   r=[t32, skb], w=[kf])
                op("scalar", lambda e: e.copy(out=kf[:, 1, g * 4:(g + 1) * 4, :], in_=t32[:, 1]), r=[t32], w=[kf])
                yield
            left = [4]

            def wrapped(g):
                yield from chain(g)
                left[0] -= 1
                if left[0] == 0:
                    dma("sync", KFs[o, :, :, cb * 16:(cb + 1) * 16, :], kf[:], r=[kf], key=kf)
            return [wrapped(g) for g in range(4)]

        WIDTH = lim.get("fftw", 4)

        def all_fchains():
            for o in range(2):
                for cb in range(HYW // 16):
                    for c_ in filt_block(o, cb):
                        yield c_
        interleave(all_fchains(), WIDTH)
        kb.end_phase()

    if "hyena" in phases:
        kb.begin_phase()
        fft = FFT()
        vr = kb.rot("v", 3, [64, 16, 128], BF16)
        x1r = kb.rot("x1", 3, [64, 16, 128], BF16)
        x2r = kb.rot("x2", 3, [64, 16, 128], BF16)
        k0r = kb.rot("k0", 2, [128, 2, 16, 128], BF16)
        k1r = kb.rot("k1", 2, [128, 2, 16, 128], BF16)
        hyr = kb.rot("hy", 3, [64, 16, 128], BF16)
        zr = kb.rot("z", 4, [64, 4, 128], BF16)
        yrr = kb.rot("yr", 4, [128, 4, 128], BF16)
        yir = kb.rot("yi", 4, [128, 4, 128], BF16)

        def ld(buf, row0):
            dma("sync", buf[:], Us[row0:row0 + 16, 1:SEQ + 1].rearrange("c (a b) -> a c b", b=128), w=[buf], key=buf)

        WIDTH = lim.get("fftw", 4)

        def conv(src_buf, src_ap, kt, g):
            pr2 = yield from fft.forward([(src_buf, src_ap, tCS, tCS[0:64, :])])
            yr_, yi_ = yrr.next(), yir.next()
            kre = kt[:, 0, g * 4:(g + 1) * 4, :].unsqueeze(1).to_broadcast([128, 2, 4, 128])
            kim = kt[:, 1, g * 4:(g + 1) * 4, :].unsqueeze(1).to_broadcast([128, 2, 4, 128])
            fft.cmul(pr2, 2, kre, kim, [kt], yr_[:], yi_[:], [yr_], [yi_])
            yield
            bank = yield from fft.inverse(yr_, yi_)
            return bank

        def hy_block(cb):
            v, x1, x2 = vr.next(), x1r.next(), x2r.next()
            k0, k1 = k0r.next(), k1r.next()
            ld(v, cb * 16); ld(x1, 512 + cb * 16); ld(x2, 1024 + cb * 16)
            dma("sync", k0[:], KFs[0, :, :, cb * 16:(cb + 1) * 16, :], w=[k0], key=k0)
            dma("sync", k1[:], KFs[1, :, :, cb * 16:(cb + 1) * 16, :], w=[k1], key=k1)
            hy = hyr.next()

            def chain(g):
                bank = yield from conv(v, v[:, g * 4:(g + 1) * 4, :], k0, g)
                z = zr.next()
                op("vector", lambda e: e.tensor_tensor(
                    out=z[:].rearrange("p c k -> p (c k)"), in0=bank[0:64, :], in1=x1[:, g * 4:(g + 1) * 4, :].rearrange("p c k -> p (c k)"), op=ALU.mult),
                   r=[bank, x1], w=[z])
                yield
                bank2 = yield from conv(z, z[:, :, :], k1, g)
                op("vector", lambda e: e.tensor_tensor(
                    out=hy[:, g * 4:(g + 1) * 4, :].rearrange("p c k -> p (c k)"), in0=bank2[0:64, :],
                    in1=x2[:, g * 4:(g + 1) * 4, :].rearrange("p c k -> p (c k)"), op=ALU.mult), r=[bank2, x2], w=[hy])
                yield
            left = [4]

            def wrapped(g):
                yield from chain(g)
                left[0] -= 1
                if left[0] == 0:
                    dma("sync", HYs[:, cb * 16:(cb + 1) * 16, :], hy[:], r=[hy], key=hy)
            return [wrapped(g) for g in range(4)]

        def all_chains():
            for cb in range(HYW // 16):
                for c_ in hy_block(cb):
                    yield c_
        interleave(all_chains(), WIDTH)
        kb.end_phase()


    XMs = kb.scratch("XMs", [SEQ, D], F32)
    H2Ts = kb.scratch("H2Ts", [D, SEQ], BF16)
    WTs = kb.scratch("WTs", [2, NE, SEQ], BF16)
    if "merge" in phases:
        wba_d = kb.inp("w_ba", [512, D]); wbh_d = kb.inp("w_bh", [512, D]); wout_d = kb.inp("w_out", [D, D])
        wr_d = kb.inp("w_router", [D, NE]); rb_d = kb.inp("rbias_b", [128, NE])
        kb.begin_phase()
        wba = kb.sb("wba", [128, 4, D], BF16); wbh = kb.sb("wbh", [128, 4, D], BF16); wout = kb.sb("wout", [128, 8, D], BF16)
        wr = kb.sb("wr", [128, 8, NE], F32); rbb = kb.sb("rbb", [128, NE], F32)
        G2 = kb.sb("G2", [128, D], F32); A2 = kb.sb("A2", [128, D], F32); B2 = kb.sb("B2", [128, D], F32)
        dma("gpsimd", wba[:], wba_d.rearrange("(k p) n -> p k n", p=128), w=[wba], key=wba)
        dma("gpsimd", wbh[:], wbh_d.rearrange("(k p) n -> p k n", p=128), w=[wbh], key=wbh)
        for k in range(8):
            dma("gpsimd", wout[:, k, :], wout_d[k * 128:(k + 1) * 128, :], w=[wout], key=wout)
        dma("sync", wr[:], wr_d.rearrange("(k p) n -> p k n", p=128), w=[wr], key=wr)
        dma("sync", rbb[:], rb_d, w=[rbb], key=rbb)
        for t_, i_ in ((G2, 2), (A2, 4), (B2, 3)):
            dma("sync", t_[:], MODS[i_], w=[t_], key=t_)
        atr = kb.rot("at", 2, [128, 4, 512], BF16)
        hytr = kb.rot("hyt", 2, [128, 4, 512], BF16)
        gar = kb.rot("ga", 2, [128, 16, 512], BF16)
        t1r = kb.rot("t1", 2, [128, 512], F32)
        t2r = kb.rot("t2", 2, [128, 512], F32)
        yTr = kb.rot("yT", 2, [128, 8, 512], BF16)
        xr = kb.rot("x", 2, [128, D], F32)
        tmr = kb.rot("tm", 3, [128, D], F32)
        xmr = kb.rot("xm", 2, [128, D], F32)
        jkr = kb.rot("jk", 1, [128, D], BF16)
        sttr = kb.rot("stt", 4, [128, 4], F32)
        h2r = kb.rot("h2", 2, [128, D], F32)
        h2Tr = kb.rot("h2T", 2, [128, 8, 128], F32)
        h2Tbr = kb.rot("h2Tb", 2, [128, 8, 128], BF16)
        rtr = kb.rot("rt", 2, [128, 8, NE], F32)
        smr = kb.rot("sm", 2, [128, 64], F32)
        wtr = kb.rot("wt", 2, [64, 2, 128], BF16)
        wlr = kb.rot("wl", 2, [64, 128], F32)
        HY_v = HYs.rearrange("a c b -> c a b")
        BIG = 1.0e9

        for ci in range(lim.get("merge", SEQ // 512)):
            t0 = ci * 512
            at, hyt, ga = atr.next(), hytr.next(), gar.next()
            dma("sync", at[:], ATs[:, t0:t0 + 512].rearrange("(k p) t -> p k t", p=128), w=[at], key=at)
            for k in range(4):
                dma("sync", hyt[:, k, :].rearrange("p (a b) -> p a b", b=128), HY_v[k * 128:(k + 1) * 128, ci * 4:(ci + 1) * 4, :], w=[hyt], key=hyt)
            dma("sync", ga[:], Gs[:, t0:t0 + 512].rearrange("(k p) t -> p k t", p=128), w=[ga], key=ga)
            yT = yTr.next()
            for f in range(8):
                bA, bH = prot.next(), prot.next()
                for k in range(4):
                    op("tensor", lambda e, bA=bA, k=k, f=f, at=at: e.matmul(bA[:, :], lhsT=wba[:, k, f * 128:(f + 1) * 128], rhs=at[:, k, :],
                                                                         start=(k == 0), stop=(k == 3)), r=[wba, at], w=[bA], inc=(k == 3))
                for k in range(4):
                    op("tensor", lambda e, bH=bH, k=k, f=f, hyt=hyt: e.matmul(bH[:, :], lhsT=wbh[:, k, f * 128:(f + 1) * 128], rhs=hyt[:, k, :],
                                                                           start=(k == 0), stop=(k == 3)), r=[wbh, hyt], w=[bH], inc=(k == 3))
                t1, t2 = t1r.next(), t2r.next()
                op("vector", lambda e, t1=t1, bA=bA, ga=ga, f=f: e.tensor_tensor(out=t1[:], in0=bA[:, :], in1=ga[:, f, :], op=ALU.mult), r=[bA, ga], w=[t1])
                op("vector", lambda e, t2=t2, bH=bH, ga=ga, f=f: e.tensor_tensor(out=t2[:], in0=bH[:, :], in1=ga[:, 8 + f, :], op=ALU.mult), r=[bH, ga], w=[t2])
                op("gpsimd", lambda e, t1=t1, t2=t2, yT=yT, f=f: e.tensor_tensor(out=yT[:, f, :], in0=t1[:], in1=t2[:], op=ALU.add), r=[t1, t2], w=[yT])
            for i in range(4):
                tok = t0 + i * 128
                xt = xr.next()
                dma("sync", xt[:], x_d[tok:tok + 128, :], w=[xt], key=xt)
                xm = xmr.next()
                tm = tmr.next()
                for dh in range(2):
                    bank = prot.next()
                    for f in range(8):
                        op("tensor", lambda e, bank=bank, f=f, i=i, dh=dh, yT=yT: e.matmul(
                            bank[:, :], lhsT=yT[:, f, i * 128:(i + 1) * 128], rhs=wout[:, f, dh * 512:(dh + 1) * 512], start=(f == 0), stop=(f == 7)),
                           r=[yT, wout], w=[bank], inc=(f == 7))
                    op("vector", lambda e, tm=tm, bank=bank, dh=dh: e.tensor_tensor(out=tm[:, dh * 512:(dh + 1) * 512], in0=bank[:, :],
                                                                                  in1=G2[:, dh * 512:(dh + 1) * 512], op=ALU.mult), r=[bank, G2], w=[tm])
                op("gpsimd", lambda e, xm=xm, tm=tm, xt=xt: e.tensor_tensor(out=xm[:], in0=tm[:], in1=xt[:], op=ALU.add), r=[tm, xt], w=[xm])
                dma("sync", XMs[tok:tok + 128, :], xm[:], r=[xm], key=xm)
                if lim.get("mstage", 9) < 2:
                    continue
                jk, st = jkr.next(), sttr.next()
                op("scalar", lambda e, jk=jk, xm=xm, st=st: e.activation(out=jk[:], in_=xm[:], func=AF.Square, accum_out=st[:, 0:1]), r=[xm], w=[jk, st])
                op("scalar", lambda e, st=st: e.activation(out=st[:, 1:2], in_=st[:, 0:1], func=AF.Sqrt, scale=1.0 / D, bias=EPS), r=[st], w=[st])
                op("vector", lambda e, st=st: e.reciprocal(out=st[:, 2:3], in_=st[:, 1:2]), r=[st], w=[st])
                h2 = h2r.next()
                h2a = tmr.next()
                op("vector", lambda e, h2a=h2a, xm=xm, st=st: e.scalar_tensor_tensor(out=h2a[:], in0=xm[:], scalar=st[:, 2:3], in1=A2[:],
                                                                                    op0=ALU.mult, op1=ALU.mult), r=[xm, st, A2], w=[h2a])
                op("gpsimd", lambda e, h2=h2, h2a=h2a: e.tensor_tensor(out=h2[:], in0=h2a[:], in1=B2[:], op=ALU.add), r=[h2a, B2], w=[h2])
                if lim.get("msub", 9) < 2:
                    continue
                h2T, h2Tb = h2Tr.next(), h2Tbr.next()
                for half in range(2):
                    bank = prot.next()
                    for kk in range(4):
                        k = half * 4 + kk
                        op("tensor", lambda e, bank=bank, kk=kk, k=k, h2=h2: e.transpose(
                            out=bank[:, kk * 128:(kk + 1) * 128], in_=h2[:, k * 128:(k + 1) * 128], identity=ident[:]),
                           r=[h2, ident], w=[bank], inc=True)
                    op("vector", lambda e, bank=bank, half=half, h2T=h2T: e.tensor_copy(
                        out=h2T[:, half * 4:half * 4 + 4, :], in_=bank[:, :].rearrange("p (k t) -> p k t", t=128)), r=[bank], w=[h2T])
                    op("gpsimd", lambda e, half=half, h2T=h2T, h2Tb=h2Tb: e.tensor_copy(
                        out=h2Tb[:, half * 4:half * 4 + 4, :], in_=h2T[:, half * 4:half * 4 + 4, :]), r=[h2T], w=[h2Tb])
                if lim.get("msub", 9) >= 3:
                    dma("sync", H2Ts[:, tok:tok + 128].rearrange("(k p) t -> p k t", p=128), h2Tb[:], r=[h2Tb], key=h2Tb)
                if lim.get("mstage", 9) < 3:
                    continue
                bank = prot.next()
                for k in range(8):
                    op("tensor", lambda e, bank=bank, k=k, h2T=h2T: e.matmul(bank[:, 0:NE], lhsT=h2T[:, k, :], rhs=wr[:, k, :], start=(k == 0), stop=(k == 7)),
                       r=[h2T, wr], w=[bank], inc=(k == 7))
                rt, sm = rtr.next(), smr.next()
                sc, sel, eq, sel2, selm, em, w_, wt = (rt[:, j, :] for j in range(8))
                m1, m2, gs, g8, gm, pen, e8, ws = (sm[:, 8 * j:8 * j + 8] for j in range(8))
                V = lambda fn, r_, w__: op("vector", fn, r=r_, w=w__)
                op("scalar", lambda e, sc=sc, bank=bank: e.activation(out=sc, in_=bank[:, 0:NE], func=AF.Sigmoid), r=[bank], w=[rt])
                V(lambda e, sel=sel, sc=sc: e.tensor_tensor(out=sel, in0=sc, in1=rbb[:], op=ALU.add), [rt, rbb], [rt])
                g3 = lambda a: a.rearrange("p (g e) -> p g e", e=8)
                V(lambda e, m1=m1, sel=sel: e.tensor_reduce(out=m1, in_=g3(sel), axis=AX.X, op=ALU.max), [rt], [sm])
                V(lambda e, eq=eq, sel=sel, m1=m1: e.tensor_tensor(out=g3(eq), in0=g3(sel), in1=m1.unsqueeze(2).to_broadcast([128, 8, 8]), op=ALU.is_equal), [rt, sm], [rt])
                V(lambda e, sel2=sel2, eq=eq, sel=sel: e.scalar_tensor_tensor(out=sel2, in0=eq, scalar=-BIG, in1=sel, op0=ALU.mult, op1=ALU.add), [rt], [rt])
                V(lambda e, m2=m2, sel2=sel2: e.tensor_reduce(out=m2, in_=g3(sel2), axis=AX.X, op=ALU.max), [rt], [sm])
                V(lambda e, gs=gs, m1=m1, m2=m2: e.tensor_tensor(out=gs, in0=m1, in1=m2, op=ALU.add), [sm], [sm])
                V(lambda e, g8=g8, gs=gs: e.max(out=g8, in_=gs), [sm], [sm])
                V(lambda e, gm=gm, gs=gs, g8=g8: e.tensor_scalar(out=gm, in0=gs, scalar1=g8[:, 3:4], scalar2=None, op0=ALU.is_ge), [sm], [sm])
                V(lambda e, pen=pen, gm=gm: e.tensor_scalar(out=pen, in0=gm, scalar1=-1.0, scalar2=BIG, op0=ALU.add, op1=ALU.mult), [sm], [sm])
                V(lambda e, selm=selm, sel=sel, pen=pen: e.tensor_tensor(out=g3(selm), in0=g3(sel), in1=pen.unsqueeze(2).to_broadcast([128, 8, 8]), op=ALU.add), [rt, sm], [rt])
                V(lambda e, e8=e8, selm=selm: e.max(out=e8, in_=selm), [rt], [sm])
                V(lambda e, em=em, selm=selm, e8=e8: e.tensor_scalar(out=em, in0=selm, scalar1=e8[:, 7:8], scalar2=None, op0=ALU.is_ge), [rt, sm], [rt])
                V(lambda e, w_=w_, sc=sc, em=em: e.tensor_tensor(out=w_, in0=sc, in1=em, op=ALU.mult), [rt], [rt])
                V(lambda e, ws=ws, w_=w_: e.tensor_reduce(out=ws[:, 0:1], in_=w_, axis=AX.X, op=ALU.add), [rt], [sm])
                V(lambda e, ws=ws: e.reciprocal(out=ws[:, 1:2], in_=ws[:, 0:1]), [sm], [sm])
                V(lambda e, wt=wt, w_=w_, ws=ws: e.tensor_scalar(out=wt, in0=w_, scalar1=ws[:, 1:2], scalar2=2.5, op0=ALU.mult, op1=ALU.mult), [rt, sm], [rt])
                if lim.get("mstage", 9) < 4:
                    continue
                bank = prot.next()
                op("tensor", lambda e, bank=bank, wt=wt: e.transpose(out=bank[0:NE, 0:128], in_=wt, identity=ident[:]), r=[rt, ident], w=[bank])
                wtb, wl = wtr.next(), wlr.next()
                op("scalar", lambda e, wtb=wtb, bank=bank: e.copy(out=wtb[:, 0, :], in_=bank[0:NE, 0:128]), r=[bank], w=[wtb])
                op("vector", lambda e, wl=wl, bank=bank, wtb=wtb: e.tensor_tensor(out=wl[:], in0=bank[0:NE, 0:128], in1=wtb[:, 0, :], op=ALU.subtract), r=[bank, wtb], w=[wl])
                op("gpsimd", lambda e, wl=wl, wtb=wtb: e.tensor_copy(out=wtb[:, 1, :], in_=wl[:]), r=[wl], w=[wtb])
                dma("sync", WTs[:, :, tok:tok + 128].rearrange("h e t -> e h t"), wtb[:], r=[wtb], key=wtb)
        kb.end_phase()


    if "moe" in phases:
        wsg_d = kb.inp("w_sh_gate", [D, EFF]); wsu_d = kb.inp("w_sh_up", [D, EFF]); wsd_d = kb.inp("w_sh_down", [EFF, D])
        out_d = kb.nc.dram_tensor("out", [SEQ, D], F32, kind="ExternalOutput").ap()
        kb.begin_phase()
        if not converted[0]:
            convert_expert_weights()
        wsh = kb.sb("wsh", [128, 8, 2 * EFF], BF16)
        wshd = kb.sb("wshd", [128, 2, D], BF16)
        dma("gpsimd", wsh[:, :, 0:EFF], wsg_d.rearrange("(k p) f -> p k f", p=128), w=[wsh], key=wsh)
        dma("gpsimd", wsh[:, :, EFF:2 * EFF], wsu_d.rearrange("(k p) f -> p k f", p=128), w=[wsh], key=wsh)
        dma("gpsimd", wshd[:], wsd_d.rearrange("(k p) d -> p k d", p=128), w=[wshd], key=wshd)
        G5 = kb.sb("G5", [128, D], F32); FN = kb.sb("FN", [128, D], F32)
        dma("sync", G5[:], MODS[5], w=[G5], key=G5)
        dma("sync", FN[:], fn_d, w=[FN], key=FN)
        Esel = kb.sb("Esel", [NE, NE, 128], BF16)
        op("vector", lambda e: e.tensor_copy(out=Esel[:], in_=identb[0:NE, 0:NE].unsqueeze(2).to_broadcast([NE, NE, 128])), r=[identb], w=[Esel])
        S.barrier()
        GE = 4
        h2Tcr = kb.rot("h2Tc", 1, [128, 8, 1024], BF16)
        wtcr = kb.rot("wtc", 1, [NE, 1024], BF16)
        acc = kb.sb("acc", [128, 8, D], F32)
        wgur = kb.rot("wgu", 3, [128, 2, 8 * EFF], BF16)
        wdr = kb.rot("wd", 8, [128, 2 * D], BF16)
        aT = kb.sb("aT", [128, GE, 2, 1024], BF16)
        bcsr = kb.rot("bcs", 4, [128, 512], BF16)
        ssr = kb.rot("ss", 2, [128, 512], BF16)
        ttr = kb.rot("tt", 2, [128, 512], BF16)
        xmr2 = kb.rot("xm2", 2, [128, D], F32)
        fr1 = kb.rot("f1", 1, [128, D], F32)
        fr2 = kb.rot("f2", 1, [128, D], F32)
        jk2 = kb.rot("jk2", 1, [128, D], BF16)
        st2 = kb.rot("st2", 4, [128, 4], F32)
        outr = kb.rot("outt", 2, [128, D], F32)
        groups = [list(range(g0, min(g0 + GE, ne_lim))) for g0 in range(0, ne_lim, GE)] + [["sh"]]
        nchunk_moe = lim.get("moe", SEQ // 1024)
        wseq = [(ch_, e_) for ch_ in range(nchunk_moe) for grp_ in groups for e_ in grp_ if e_ != "sh"]
        wloaded = {}
        wptr = [0]

        def prefetch_w(upto):
            while wptr[0] < min(upto, len(wseq)):
                e_ = wseq[wptr[0]][1]
                wgu, wd = wgur.next(), wdr.next()
                dma("sync", wgu[:, 0, :], EWg[e_], w=[wgu], key=wgu)
                dma("sync", wgu[:, 1, :], EWu[e_], w=[wgu], key=wgu)
                dma("sync", wd[:], EWd[e_], w=[wd], key=wd)
                wloaded[wptr[0]] = (wgu, wgu[:, 0, :].rearrange("p (k f) -> p k f", k=8), wgu[:, 1, :].rearrange("p (k f) -> p k f", k=8),
                                    wd, wd[:].rearrange("p (k d) -> p k d", k=2))
                wptr[0] += 1

        def load_chunk(ch_):
            h2Tc_, wtc_ = h2Tcr.next(), wtcr.next()
            for k in range(8):
                dma("sync", h2Tc_[:, k, :], H2Ts[k * 128:(k + 1) * 128, ch_ * 1024:(ch_ + 1) * 1024], w=[h2Tc_], key=h2Tc_)
            dma("sync", wtc_[:], WTs[0, :, ch_ * 1024:(ch_ + 1) * 1024], w=[wtc_], key=wtc_)
            return h2Tc_, wtc_

        chunk_bufs = {0: load_chunk(0)}
        widx = 0
        for ch in range(nchunk_moe):
            t0 = ch * 1024
            h2Tc, wtc = chunk_bufs.pop(ch)
            for gi, grp in enumerate(groups):
                wts = []
                for ei, e_ in enumerate(grp):
                    if e_ == "sh":
                        wts.append((wsh, wsh[:, :, 0:EFF], wsh[:, :, EFF:2 * EFF], wshd, wshd[:]))
                    else:
                        prefetch_w(widx + 3)
                        wts.append(wloaded.pop(widx))
                        widx += 1
                    wgub, wgv, wuv, wdb, wdv = wts[ei]
                    for half in range(2):
                        hs = slice(half * 512, half * 512 + 512)
                        if e_ != "sh":
                            bk = prot.next()
                            op("tensor", lambda e, bk=bk, e_=e_, wtc=wtc, hs=hs: e.matmul(bk[:, :], lhsT=Esel[:, e_, :], rhs=wtc[:, hs], start=True, stop=True),
                               r=[Esel, wtc], w=[bk])
                            bcs = bcsr.next()
                            op("scalar", lambda e, bcs=bcs, bk=bk: e.copy(out=bcs[:], in_=bk[:, :]), r=[bk], w=[bcs])
                        for f in range(2):
                            bg, bu = prot.next(), prot.next()
                            for (bank, wv) in ((bg, wgv), (bu, wuv)):
                                for k in range(8):
                                    op("tensor", lambda e, bank=bank, wv=wv, k=k, f=f, h2Tc=h2Tc, hs=hs: e.matmul(
                                        bank[:, :], lhsT=wv[:, k, f * 128:(f + 1) * 128], rhs=h2Tc[:, k, hs], start=(k == 0), stop=(k == 7)),
                                       r=[wgub, h2Tc], w=[bank], inc=(k == 7))
                            ss = ssr.next()
                            op("scalar", lambda e, ss=ss, bg=bg: e.activation(out=ss[:], in_=bg[:, :], func=AF.Silu), r=[bg], w=[ss])
                            if e_ == "sh":
                                op("vector", lambda e, bu=bu, ss=ss, ei=ei, f=f, hs=hs: e.tensor_tensor(out=aT[:, ei, f, hs], in0=bu[:, :], in1=ss[:], op=ALU.mult),
                                   r=[bu, ss], w=[aT])
                            else:
                                tt = ttr.next()
                                op("vector", lambda e, tt=tt, bu=bu, ss=ss: e.tensor_tensor(out=tt[:], in0=bu[:, :], in1=ss[:], op=ALU.mult), r=[bu, ss], w=[tt])
                                op("gpsimd", lambda e, tt=tt, bcs=bcs, ei=ei, f=f, hs=hs: e.tensor_tensor(out=aT[:, ei, f, hs], in0=tt[:], in1=bcs[:], op=ALU.mult),
                                   r=[tt, bcs], w=[aT])
                for ti in range(8):
                    for dh in range(2):
                        bank = prot.next()
                        nmm = len(grp) * 2
                        j = 0
                        for ei in range(len(grp)):
                            wdb, wdv = wts[ei][3], wts[ei][4]
                            for f in range(2):
                                op("tensor", lambda e, bank=bank, ei=ei, f=f, ti=ti, dh=dh, wdv=wdv, j=j, nmm=nmm: e.matmul(
                                    bank[:, :], lhsT=aT[:, ei, f, ti * 128:(ti + 1) * 128], rhs=wdv[:, f, dh * 512:(dh + 1) * 512],
                                    start=(j == 0), stop=(j == nmm - 1)), r=[aT, wdb], w=[bank], inc=(j == nmm - 1))
                                j += 1
                        dsl = slice(dh * 512, dh * 512 + 512)
                        if gi == 0:
                            op("scalar", lambda e, bank=bank, ti=ti, dsl=dsl: e.copy(out=acc[:, ti, dsl], in_=bank[:, :]), r=[bank], w=[acc])
                        else:
                            op("vector", lambda e, bank=bank, ti=ti, dsl=dsl: e.tensor_tensor(out=acc[:, ti, dsl], in0=bank[:, :], in1=acc[:, ti, dsl], op=ALU.add),
                               r=[bank, acc], w=[acc])
            if ch + 1 < nchunk_moe:
                chunk_bufs[ch + 1] = load_chunk(ch + 1)
                prefetch_w(widx + 3)
            for ti in range(8):
                tok = t0 + ti * 128
                xm = xmr2.next()
                dma("sync", xm[:], XMs[tok:tok + 128, :], w=[xm], key=xm)
                f1, f2 = fr1.next(), fr2.next()
                op("vector", lambda e, f1=f1, ti=ti: e.tensor_tensor(out=f1[:], in0=acc[:, ti, :], in1=G5[:], op=ALU.mult), r=[acc, G5], w=[f1])
                op("gpsimd", lambda e, f1=f1, f2=f2, xm=xm: e.tensor_tensor(out=f2[:], in0=f1[:], in1=xm[:], op=ALU.add), r=[f1, xm], w=[f2])
                jk, st = jk2.next(), st2.next()
                op("scalar", lambda e, jk=jk, f2=f2, st=st: e.activation(out=jk[:], in_=f2[:], func=AF.Square, accum_out=st[:, 0:1]), r=[f2], w=[jk, st])
                op("scalar", lambda e, st=st: e.activation(out=st[:, 1:2], in_=st[:, 0:1], func=AF.Sqrt, scale=1.0 / D, bias=EPS), r=[st], w=[st])
                op("vector", lambda e, st=st: e.reciprocal(out=st[:, 2:3], in_=st[:, 1:2]), r=[st], w=[st])
                ot = outr.next()
                op("vector", lambda e, ot=ot, f2=f2, st=st: e.scalar_tensor_tensor(out=ot[:], in0=f2[:], scalar=st[:, 2:3], in1=FN[:], op0=ALU.mult, op1=ALU.mult),
                   r=[f2, st, FN], w=[ot])
                dma("sync", out_d[tok:tok + 128, :], ot[:], r=[ot], key=ot)
        kb.end_phase()

    return kb


_ROPE_PERM = np.array(list(range(8, 16)) + list(range(0, 8)) + list(range(24, 32)) + list(range(16, 24)))


def _rope_tables():
    half = 16
    inv_freq = (np.float32(10000.0) ** (-np.arange(0, half, 2, dtype=np.float32) / np.float32(half))).astype(np.float32)
    rows = SEQ // 64
    row = np.broadcast_to(np.arange(rows, dtype=np.float32)[:, None], (rows, 64)).reshape(-1)
    col = np.broadcast_to(np.arange(64, dtype=np.float32)[None, :], (rows, 64)).reshape(-1)
    ang_r = (row[:, None] * inv_freq).astype(np.float32)
    ang_c = (col[:, None] * inv_freq).astype(np.float32)
    C = np.zeros((32, SEQ), np.float32)
    Sg = np.zeros((32, SEQ), np.float32)
    C[0:8] = np.cos(ang_r).T; C[8:16] = np.cos(ang_r).T
    C[16:24] = np.cos(ang_c).T; C[24:32] = np.cos(ang_c).T
    Sg[0:8] = -np.sin(ang_r).T; Sg[8:16] = np.sin(ang_r).T
    Sg[16:24] = -np.sin(ang_c).T; Sg[24:32] = np.sin(ang_c).T
    return C, Sg


def _colsT(v, n):
    return np.ascontiguousarray(np.asarray(v, np.float32).reshape(n, 128).T)


def prep_shared(inp):
    g = lambda k: np.asarray(inp[k][0], np.float32)
    out = {}
    out["cctxT"] = _colsT(inp["c_ctx"], 8)
    out["w_mod"] = g("w_mod")
    out["b_mod"] = g("b_mod")[None, :]
    out["nmix_b"] = np.ascontiguousarray(np.broadcast_to(g("norm_mix")[None, :], (128, D)))
    out["nffn_b"] = np.ascontiguousarray(np.broadcast_to(g("norm_ffn")[None, :], (128, D)))
    out["fn_b"] = np.ascontiguousarray(np.broadcast_to(np.asarray(inp["final_norm"], np.float32)[None, :], (128, D)))
    w_in = g("w_in"); b_in = g("b_in")
    out["w_in"] = w_in
    binT = np.zeros((128, 33), np.float32)
    binT[:, 0:2] = _colsT(b_in[C_Q:C_Q + 256], 2)
    binT[:, 2:3] = _colsT(b_in[C_KV:C_KV + 128], 1)
    binT[:, 3:15] = _colsT(b_in[C_HY:C_HY + 1536], 12)
    binT[:, 15:31] = _colsT(b_in[C_GATE:C_GATE + 2048], 16)
    binT[0:32, 31] = b_in[C_KPE:C_KPE + 32]
    binT[0:32, 32] = b_in[C_KPE:C_KPE + 32][_ROPE_PERM]
    out["b_inT"] = binT
    out["w_kpe_sw"] = np.ascontiguousarray(w_in[:, C_KPE:C_KPE + 32][:, _ROPE_PERM])
    C, Sg = _rope_tables()
    out["ropeC"] = C; out["ropeS"] = Sg
    out["q_normT"] = _colsT(g("q_norm"), 2)
    out["kv_normT"] = _colsT(g("kv_norm"), 1)
    w_uq = g("w_uq")
    out["w_uq"] = w_uq
    wsw = w_uq.reshape(256, H, DQ).copy()
    wsw[:, :, 64:96] = wsw[:, :, 64:96][:, :, _ROPE_PERM]
    out["w_uq_sw"] = np.ascontiguousarray(wsw.reshape(256, H * DQ))
    wkv = g("w_ukv").reshape(128, H, 128)
    out["w_ukv_k"] = np.ascontiguousarray(wkv[:, :, 0:64].reshape(128, 512))
    out["w_ukv_v"] = np.ascontiguousarray(wkv[:, :, 64:128].reshape(128, 512))
    hcw = g("hy_conv_w")
    t = np.zeros((128, 36), np.float32)
    for j in range(12):
        for k in range(3):
            t[:, 3 * j + k] = hcw[k, j * 128:(j + 1) * 128]
    out["hy_conv_wT"] = t
    out["hy_conv_bT"] = _colsT(g("hy_conv_b"), 12)
    out["ident"] = np.eye(128, dtype=np.float32)
    jk = np.outer(np.arange(128), np.arange(128)).astype(np.float64)
    out["dftC"] = np.cos(2 * np.pi * jk / 128).astype(np.float32)
    out["dftS"] = np.sin(2 * np.pi * jk / 128).astype(np.float32)
    out["twC"] = np.cos(2 * np.pi * jk / NFFT).astype(np.float32)
    out["twS"] = np.sin(2 * np.pi * jk / NFFT).astype(np.float32)
    L = SEQ
    t = np.linspace(0.0, 1.0, L, dtype=np.float32)
    w = (np.float32(2.0 * math.pi) * np.arange(L, dtype=np.float32) / np.float32(L)).astype(np.float32)
    f = np.linspace(1e-4, 7.0, 8, dtype=np.float32)[None, :]
    fw = (f * w[:, None]).astype(np.float32)
    emb = np.concatenate([t[:, None], np.cos(fw), -np.sin(fw)], axis=-1).astype(np.float32)
    ridx = np.concatenate([[0], L - np.arange(1, L)])
    out["embF"] = np.ascontiguousarray(emb.T)
    out["embB"] = np.ascontiguousarray(emb[np.minimum(ridx, L - 1)].T)
    tB = t[np.minimum(ridx, L - 1)].copy(); tB[0] = 1e4
    out["trowF"] = np.ascontiguousarray(np.broadcast_to(t[None, :], (128, L)))
    out["trowB"] = np.ascontiguousarray(np.broadcast_to(tB[None, :], (128, L)))
    deltas = np.abs(np.linspace(math.log(1e-2) / 1.5, math.log(1e-2) / 0.3, HYW, dtype=np.float32))
    out["negdelta"] = np.ascontiguousarray(-deltas.reshape(4, 128).T)
    out["hf_w1"] = g("hy_filt_w1"); out["hf_w2"] = g("hy_filt_w2"); out["hf_w3"] = g("hy_filt_w3")
    out["hf_vecs"] = np.ascontiguousarray(np.stack([g("hy_filt_b1"), g("hy_filt_b2"), g("hy_filt_freq")], axis=1))
    out["skip_b"] = np.ascontiguousarray(np.broadcast_to(g("hy_skip")[None], (128, 2, HYW)))
    out["w_ba"] = g("w_branch_attn"); out["w_bh"] = g("w_branch_hyena"); out["w_out"] = g("w_out")
    out["w_router"] = g("w_router")
    out["rbias_b"] = np.ascontiguousarray(np.broadcast_to(g("router_bias")[None], (128, NE)))
    out["w_exp_gate"] = g("w_exp_gate"); out["w_exp_up"] = g("w_exp_up"); out["w_exp_down"] = g("w_exp_down")
    out["w_sh_gate"] = g("w_sh_gate"); out["w_sh_up"] = g("w_sh_up"); out["w_sh_down"] = g("w_sh_down")
    return out


def prep_core(inp, b):
    return {
        "x": np.ascontiguousarray(np.asarray(inp["x"][b], np.float32)),
        "ctx": np.ascontiguousarray(np.asarray(inp["ctx"][b], np.float32)),
        "cT": _colsT(inp["c"][b], 8),
    }


_PROGRAM = None


def kernel(**inputs):
    global _PROGRAM
    if _PROGRAM is None:
        _PROGRAM = build()
    kb = _PROGRAM
    shared = prep_shared(inputs)
    in_maps = []
    for b in range(8):
        allin = dict(shared)
        allin.update(prep_core(inputs, b))
        in_maps.append({k: allin[k] for k in kb.inputs})
    res = run_bass_kernel_spmd(kb.nc, in_maps, core_ids=list(range(8)))
    out = np.stack([np.asarray(r["out"], dtype=np.float32) for r in res.results], axis=0)
    return out
```

```python
import math
from contextlib import ExitStack
import numpy as np
import concourse.bass as bass
import concourse.mybir as mybir
from concourse.bass_utils import run_bass_kernel_spmd

F32 = mybir.dt.float32
BF16 = mybir.dt.bfloat16
AF = mybir.ActivationFunctionType
ALU = mybir.AluOpType
AX = mybir.AxisListType

ENGS = ("tensor", "vector", "scalar", "gpsimd", "sync")

D = 1024
SEQ = 8192
NCTX = 256
NKEY = SEQ + NCTX
H = 8
DQ = 96
HYW = 512
NE = 64
EFF = 256
NFFT = 2 * SEQ
EPS = 1e-6
ATTN_SCALE = 1.0 / math.sqrt(96.0)
IN_WIDTH = 4000
C_Q, C_KV, C_KPE, C_HY, C_GATE = 0, 256, 384, 416, 1952


class Buf:
    __slots__ = ("name", "t", "last_w", "readers", "dsem")

    def __init__(self, name, t=None):
        self.name = name
        self.t = t
        self.last_w = None
        self.readers = []
        self.dsem = {}

    def __getitem__(self, k):
        return self.t[k]


class Sync:
    def __init__(self, nc):
        self.nc = nc
        self.lists = {e: [] for e in ENGS}
        self.cnt = {e: 0 for e in ENGS}
        self.esem = {e: nc.alloc_semaphore("es_" + e) for e in ENGS}
        self.known = {e: {} for e in ENGS}
        self.semvals = {}
        self.free_dsems = {"hw": [], "sw": []}
        self.n_dsems = 0
        self.ninst = 0
        for e in ENGS:
            self.semvals[id(self.esem[e])] = [self.esem[e], 0]

    def _wait(self, eng, tok):
        sem, val = tok
        k = self.known[eng]
        if k.get(id(sem), 0) >= val:
            return
        k[id(sem)] = val
        self.lists[eng].append(lambda e, sem=sem, val=val: e.wait_ge(sem, val))

    def _deps(self, eng, r, w):
        toks = []
        for b in r:
            if b.last_w is not None:
                toks.append(b.last_w)
        for b in w:
            if b.last_w is not None:
                toks.append(b.last_w)
            toks.extend(b.readers)
        pe = self.esem["tensor"]
        best = {}
        for sem, val in toks:
            if eng == "tensor" and sem is pe:
                continue
            if best.get(id(sem), (None, -1))[1] < val:
                best[id(sem)] = (sem, val)
        for tok in best.values():
            self._wait(eng, tok)

    def _mark(self, tok, r, w):
        for b in r:
            rd = b.readers
            rd.append(tok)
            if len(rd) > 16:
                best = {}
                for s, v in rd:
                    if best.get(id(s), (None, -1))[1] < v:
                        best[id(s)] = (s, v)
                b.readers = list(best.values())
        for b in w:
            b.last_w = tok
            b.readers = []

    def op(self, eng, fn, r=(), w=(), inc=True):
        self._deps(eng, r, w)
        sem = self.esem[eng]
        self.ninst += 1
        if inc:
            self.cnt[eng] += 1
            v = self.cnt[eng]
            self.semvals[id(sem)][1] = v
            self.lists[eng].append(lambda e, fn=fn, sem=sem: fn(e).then_inc(sem, 1))
            tok = (sem, v)
        else:
            self.lists[eng].append(lambda e, fn=fn: fn(e))
            tok = (sem, self.cnt[eng] + 1)
        self._mark(tok, r, w)
        return tok

    def _dsem(self, b, q):
        kind = "sw" if q == "gpsimd" else "hw"
        if kind not in b.dsem:
            if self.free_dsems[kind]:
                b.dsem[kind] = self.free_dsems[kind].pop()
            else:
                self.n_dsems += 1
                sem = self.nc.alloc_semaphore("ds%d" % self.n_dsems)
                b.dsem[kind] = sem
                self.semvals[id(sem)] = [sem, 0]
        return b.dsem[kind]

    def dma(self, q, out, in_, r=(), w=(), key=None, **kw):
        self._deps(q, r, w)
        sem = self._dsem(key, q)
        sv = self.semvals[id(sem)]
        sv[1] += 16
        v = sv[1]
        self.ninst += 1
        self.lists[q].append(
            lambda e, out=out, in_=in_, sem=sem, kw=kw: e.dma_start(out=out, in_=in_, **kw).then_inc(sem, 16))
        tok = (sem, v)
        self._mark(tok, r, w)
        return tok

    def release(self, bufs):
        for b in bufs:
            for kind, sem in b.dsem.items():
                self.free_dsems[kind].append(sem)
            b.dsem = {}

    def barrier(self):
        for e in ENGS:
            for sem, v in list(self.semvals.values()):
                if v > 0:
                    self._wait(e, (sem, v))

    def emit(self):
        nc = self.nc
        lists = self.lists
        with nc.Block() as block:
            @block.tensor
            def _(e):
                for f in lists["tensor"]:
                    f(e)

            @block.vector
            def _(e):
                for f in lists["vector"]:
                    f(e)

            @block.scalar
            def _(e):
                for f in lists["scalar"]:
                    f(e)

            @block.gpsimd
            def _(e):
                for f in lists["gpsimd"]:
                    f(e)

            @block.sync
            def _(e):
                for f in lists["sync"]:
                    f(e)
        self.lists = {e: [] for e in ENGS}


class Rot:
    def __init__(self, bufs):
        self.bufs = bufs
        self.i = 0

    def next(self):
        b = self.bufs[self.i % len(self.bufs)]
        self.i += 1
        return b


class KB:
    def __init__(self, dbg=(), dbg_in=()):
        self.nc = bass.Bass("TRN2", target_bir_lowering=False)
        self.S = Sync(self.nc)
        self.dbg = set(dbg)
        self.dbg_in = set(dbg_in)
        self.es = None
        self.phase_bufs = []
        self.inputs = {}
        self.uid = 0
        nc = self.nc
        self.P = nc.alloc_psum_tensor("P", [128, 4096], F32).ap()
        self.Pb = self.P.bitcast(BF16)
        self.pb = [Buf("pb%d" % i, self.P[:, 512 * i:512 * (i + 1)]) for i in range(8)]
        self.pbb = [self.Pb[:, 1024 * i:1024 * (i + 1)] for i in range(8)]

    def inp(self, name, shape, dt=F32):
        t = self.nc.dram_tensor(name, list(shape), dt, kind="ExternalInput").ap()
        self.inputs[name] = t
        return t

    def scratch(self, name, shape, dt):
        kind = "ExternalOutput" if name in self.dbg else "Internal"
        if name in self.dbg_in:
            kind = "ExternalInput"
        t = self.nc.dram_tensor(name, list(shape), dt, kind=kind).ap()
        if name in self.dbg_in:
            self.inputs[name] = t
        return t

    def sb(self, name, shape, dt, persist=False):
        self.uid += 1
        nm = "%s_%d" % (name, self.uid)
        if persist:
            t = self.nc.alloc_sbuf_tensor(nm, list(shape), dt).ap()
            return Buf(nm, t)
        t = self.es.enter_context(self.nc.sbuf_tensor(nm, list(shape), dt))
        b = Buf(nm, t.ap() if hasattr(t, "ap") and callable(getattr(t, "ap")) else t)
        self.phase_bufs.append(b)
        return b

    def rot(self, name, n, shape, dt):
        return Rot([self.sb("%s%d" % (name, i), shape, dt) for i in range(n)])

    def begin_phase(self):
        self.es = ExitStack()
        self.phase_bufs = []

    def end_phase(self):
        self.S.barrier()
        self.S.emit()
        self.S.release(self.phase_bufs)
        self.es.close()
        self.es = None


def _v(eng):
    return eng


def build(dbg=(), phases=("p0", "p1", "attn", "filt", "hyena", "merge", "moe"), dbg_in=(), lim=None):
    kb = KB(dbg, dbg_in)
    lim = lim or {}
    nc, S = kb.nc, kb.S
    pb = kb.pb
    op, dma = S.op, S.dma

    x_d = kb.inp("x", [SEQ, D])
    ctx_d = kb.inp("ctx", [NCTX, D])
    cT_d = kb.inp("cT", [128, 8])
    cctxT_d = kb.inp("cctxT", [128, 8])
    wmod_d = kb.inp("w_mod", [D, 6 * D])
    bmod_d = kb.inp("b_mod", [1, 6 * D])
    nmix_d = kb.inp("nmix_b", [128, D])
    nffn_d = kb.inp("nffn_b", [128, D])
    fn_d = kb.inp("fn_b", [128, D])
    win_d = kb.inp("w_in", [D, IN_WIDTH])
    binT_d = kb.inp("b_inT", [128, 33])
    wkpesw_d = kb.inp("w_kpe_sw", [D, 32])
    ropeC_d = kb.inp("ropeC", [32, SEQ])
    ropeS_d = kb.inp("ropeS", [32, SEQ])
    qnormT_d = kb.inp("q_normT", [128, 2])
    kvnormT_d = kb.inp("kv_normT", [128, 1])
    wuq_d = kb.inp("w_uq", [256, H * DQ])
    wuqsw_d = kb.inp("w_uq_sw", [256, H * DQ])
    wukvk_d = kb.inp("w_ukv_k", [128, 512])
    wukvv_d = kb.inp("w_ukv_v", [128, 512])
    hcw_d = kb.inp("hy_conv_wT", [128, 36])
    hcb_d = kb.inp("hy_conv_bT", [128, 12])
    ident_d = kb.inp("ident", [128, 128])

    MODS = kb.scratch("MODS", [8, 128, D], F32)
    Qs = kb.scratch("Qs", [H, DQ, SEQ], BF16)
    Ks = kb.scratch("Ks", [H, DQ, NKEY], BF16)
    Vs = kb.scratch("Vs", [NKEY, 512], BF16)
    Us = kb.scratch("Us", [1536, SEQ + 128], BF16)
    Gs = kb.scratch("Gs", [2048, SEQ], BF16)

    ident = kb.sb("ident", [128, 128], F32, persist=True)
    identb = kb.sb("identb", [128, 128], BF16, persist=True)
    ones = kb.sb("ones", [128, 128], F32, persist=True)
    onesb = kb.sb("onesb", [128, 128], BF16, persist=True)
    prot = Rot(pb)

    kb.begin_phase()
    dma("sync", ident[:], ident_d, w=[ident], key=ident)
    dma("gpsimd", identb[:], ident_d, w=[identb], key=identb)
    op("vector", lambda e: e.memset(ones[:], 1.0), w=[ones])
    op("vector", lambda e: e.memset(onesb[:], 1.0), w=[onesb])

    if "p0" in phases:
        cT = kb.sb("cT", [128, 16], F32)
        sc = kb.sb("sc", [128, 16], F32)
        scb = kb.sb("scb", [128, 16, 128], F32)
        bmod = kb.sb("bmod", [1, 6 * D], F32)
        nmix = kb.sb("nmix", [128, D], F32)
        nffn = kb.sb("nffn", [128, D], F32)
        wmr = kb.rot("wm", 2, [128, 8, 512], F32)
        mtr = kb.rot("mt", 3, [128, 512], F32)
        dma("sync", cT[:, 0:8], cT_d, w=[cT], key=cT)
        dma("sync", cT[:, 8:16], cctxT_d, w=[cT], key=cT)
        dma("sync", bmod[:], bmod_d, w=[bmod], key=bmod)
        dma("sync", nmix[:], nmix_d, w=[nmix], key=nmix)
        dma("sync", nffn[:], nffn_d, w=[nffn], key=nffn)
        op("scalar", lambda e: e.activation(out=sc[:], in_=cT[:], func=AF.Silu), r=[cT], w=[sc])
        op("vector", lambda e: e.tensor_copy(out=scb[:], in_=sc[:].unsqueeze(2).to_broadcast([128, 16, 128])), r=[sc], w=[scb])
        wm_v = wmod_d.rearrange("(k p) n -> p k n", p=128)
        for n in range(12):
            wm = wmr.next()
            dma("sync", wm[:], wm_v[:, :, n * 512:(n + 1) * 512], w=[wm], key=wm)
            j, half = n // 2, n % 2
            cs = slice(half * 512, half * 512 + 512)
            for which in range(2 if n < 4 else 1):
                bank = prot.next()
                for k in range(8):
                    op("tensor", lambda e, bank=bank, k=k, wm=wm, which=which: e.matmul(
                        bank[:], lhsT=scb[:, which * 8 + k, :], rhs=wm[:, k, :], start=(k == 0), stop=False),
                       r=[scb, wm], w=[bank], inc=False)
                op("tensor", lambda e, bank=bank, n=n: e.matmul(
                    bank[:], lhsT=ones[0:1, :], rhs=bmod[0:1, n * 512:(n + 1) * 512], start=False, stop=True),
                   r=[ones, bmod], w=[bank])
                mt = mtr.next()
                if j in (1, 4):
                    gb = nmix if j == 1 else nffn
                    op("vector", lambda e, mt=mt, bank=bank, gb=gb, cs=cs: e.scalar_tensor_tensor(
                        out=mt[:], in0=bank[:], scalar=1.0, in1=gb[:, cs], op0=ALU.add, op1=ALU.mult),
                       r=[bank, gb], w=[mt])
                else:
                    op("scalar", lambda e, mt=mt, bank=bank: e.copy(out=mt[:], in_=bank[:]), r=[bank], w=[mt])
                if which == 0:
                    idx = {0: 0, 1: 1, 2: 2, 3: 3, 4: 4, 5: 5}[j]
                else:
                    idx = {0: 6, 1: 7}[j]
                dma("sync", MODS[idx][:, cs], mt[:], r=[mt], key=mt)
    kb.end_phase()

    if "p1" in phases:
        kb.begin_phase()
        win = kb.sb("win", [128, 8, IN_WIDTH], BF16)
        wksw = kb.sb("wksw", [128, 8, 32], BF16)
        binT = kb.sb("binT", [128, 33], F32)
        A1 = kb.sb("A1", [128, D], F32)
        B1 = kb.sb("B1", [128, D], F32)
        Ac = kb.sb("Ac", [128, D], F32)
        Bc = kb.sb("Bc", [128, D], F32)
        qn2 = kb.sb("qn2", [128, 2], F32)
        kvn = kb.sb("kvn", [128, 1], F32)
        wuq = kb.sb("wuq", [128, 2, H * DQ], BF16)
        wuqsw = kb.sb("wuqsw", [128, 2, H * DQ], BF16)
        wkv = kb.sb("wkv", [128, 1024], BF16)
        hcw = kb.sb("hcw", [128, 36], F32)
        hcb = kb.sb("hcb", [128, 12], F32)
        ptall = kb.sb("ptall", [128, 12, 516], F32)
        win_v = win_d.rearrange("(k p) n -> p k n", p=128)
        for k in range(8):
            dma("gpsimd", win[:, k, :], win_v[:, k, :], w=[win], key=win)
        dma("gpsimd", wksw[:], wkpesw_d.rearrange("(k p) n -> p k n", p=128), w=[wksw], key=wksw)
        dma("sync", binT[:], binT_d, w=[binT], key=binT)
        for t_, i_ in ((A1, 1), (B1, 0), (Ac, 7), (Bc, 6)):
            dma("sync", t_[:], MODS[i_], w=[t_], key=t_)
        dma("sync", qn2[:], qnormT_d, w=[qn2], key=qn2)
        dma("sync", kvn[:], kvnormT_d, w=[kvn], key=kvn)
        dma("sync", hcw[:], hcw_d, w=[hcw], key=hcw)
        dma("sync", hcb[:], hcb_d, w=[hcb], key=hcb)
        op("vector", lambda e: e.memset(ptall[:], 0.0), w=[ptall])
        xtr = kb.rot("xt", 2, [128, D], F32)
        h32r = kb.rot("h32", 1, [128, D], F32)
        stg = h32r.bufs[0]
        for src_d, dst in ((wuq_d, wuq), (wuqsw_d, wuqsw)):
            for k in range(2):
                dma("sync", stg[:, 0:H * DQ], src_d[k * 128:(k + 1) * 128, :], w=[stg], key=stg)
                op("vector", lambda e, dst=dst, k=k: e.tensor_scalar(
                    out=dst[:, k, :], in0=stg[:, 0:H * DQ], scalar1=qn2[:, k:k + 1], scalar2=None, op0=ALU.mult),
                   r=[stg, qn2], w=[dst])
        dma("sync", stg[:, 0:512], wukvk_d, w=[stg], key=stg)
        dma("sync", stg[:, 512:1024], wukvv_d, w=[stg], key=stg)
        op("vector", lambda e: e.tensor_scalar(out=wkv[:], in0=stg[:], scalar1=kvn[:, 0:1], scalar2=None, op0=ALU.mult),
           r=[stg, kvn], w=[wkv])

        junkr = kb.rot("junk", 1, [128, D], BF16)
        str_ = kb.rot("st", 4, [128, 4], F32)
        hr = kb.rot("h", 2, [128, D], BF16)
        hTr = kb.rot("hT", 2, [128, 8, 512], BF16)
        qlr = kb.rot("ql", 2, [128, 3, 512], BF16)
        sqr = kb.rot("sq", 2, [128, 512], F32)
        rsr = kb.rot("rs", 1, [128, 2, 512], F32)
        rcolr = kb.rot("rcol", 2, [128, 4], F32)
        ropr = kb.rot("rop", 1, [96, 2, 512], F32)
        tmpr = kb.rot("tmp", 3, [96, 512], F32)
        qor = kb.rot("qo", 3, [96, 512], BF16)
        kor = kb.rot("ko", 3, [96, 512], BF16)
        vor = kb.rot("vo", 2, [128, 512], BF16)
        ubr = kb.rot("ub", 3, [128, 512], BF16)
        gbr = kb.rot("gb", 3, [128, 512], BF16)
        kpr = kb.rot("kp", 1, [32, 2, 512], F32)

        def proj_chunk(tok0, ntok, is_ctx):
            nt = ntok // 128
            src = ctx_d if is_ctx else x_d
            A, B = (Ac, Bc) if is_ctx else (A1, B1)
            key0 = tok0 if is_ctx else NCTX + tok0
            hT = hTr.next()
            for i in range(nt):
                xt = xtr.next()
                dma("sync", xt[:], src[tok0 + i * 128: tok0 + (i + 1) * 128, :], w=[xt], key=xt)
                junk = junkr.next()
                st = str_.next()
                op("scalar", lambda e, junk=junk, xt=xt, st=st: e.activation(
                    out=junk[:], in_=xt[:], func=AF.Square, accum_out=st[:, 0:1]), r=[xt], w=[junk, st])
                op("scalar", lambda e, st=st: e.activation(
                    out=st[:, 1:2], in_=st[:, 0:1], func=AF.Sqrt, scale=1.0 / D, bias=EPS), r=[st], w=[st])
                op("vector", lambda e, st=st: e.reciprocal(out=st[:, 2:3], in_=st[:, 1:2]), r=[st], w=[st])
                h32 = h32r.next()
                op("vector", lambda e, h32=h32, xt=xt, st=st, A=A: e.scalar_tensor_tensor(
                    out=h32[:], in0=xt[:], scalar=st[:, 2:3], in1=A[:], op0=ALU.mult, op1=ALU.mult),
                   r=[xt, st, A], w=[h32])
                hb = hr.next()
                op("gpsimd", lambda e, hb=hb, h32=h32, B=B: e.tensor_tensor(out=hb[:], in0=h32[:], in1=B[:], op=ALU.add),
                   r=[h32, B], w=[hb])
                for half in range(2):
                    bank = prot.next()
                    bankb = kb.Pb[:, bank_index(bank) * 1024: bank_index(bank) * 1024 + 512]
                    for kk in range(4):
                        k = half * 4 + kk
                        op("tensor", lambda e, bankb=bankb, kk=kk, hb=hb, k=k: e.transpose(
                            out=bankb[:, kk * 128:(kk + 1) * 128], in_=hb[:, k * 128:(k + 1) * 128], identity=identb[:]),
                           r=[hb, identb], w=[bank], inc=(kk == 3))
                    op("vector", lambda e, hT=hT, half=half, i=i, bankb=bankb: e.tensor_copy(
                        out=hT[:, half * 4:half * 4 + 4, i * 128:(i + 1) * 128],
                        in_=bankb.rearrange("p (k t) -> p k t", t=128)), r=[bank], w=[hT])

            yield

            def proj(c0, m, dst_bank, wsrc=None):
                for k in range(8):
                    lhs = win[:, k, c0:c0 + m] if wsrc is None else wsrc[:, k, :]
                    op("tensor", lambda e, lhs=lhs, k=k, dst_bank=dst_bank: e.matmul(
                        dst_bank[0:m, 0:ntok], lhsT=lhs, rhs=hT[:, k, 0:ntok], start=(k == 0), stop=(k == 7)),
                       r=[win if wsrc is None else wsrc, hT], w=[dst_bank], inc=(k == 7))

            ql = qlr.next()
            rs = rsr.next()
            rcol = rcolr.next()
            lat_tiles = ((2, C_KV, 1),) if is_ctx else ((0, C_Q, 0), (1, C_Q + 128, 0), (2, C_KV, 1))
            ssb = {0: prot.next(), 1: prot.next()}
            for (slot, c0, grp) in lat_tiles:
                bank = prot.next()
                proj(c0, 128, bank)
                bcol = {0: 0, 1: 1, 2: 2}[slot]
                op("scalar", lambda e, ql=ql, slot=slot, bank=bank, bcol=bcol: e.activation(
                    out=ql[:, slot, 0:ntok], in_=bank[:, 0:ntok], func=AF.Identity, bias=binT[:, bcol:bcol + 1]),
                   r=[bank, binT], w=[ql])
                sq = sqr.next()
                op("scalar", lambda e, sq=sq, bank=bank, bcol=bcol: e.activation(
                    out=sq[:, 0:ntok], in_=bank[:, 0:ntok], func=AF.Square, bias=binT[:, bcol:bcol + 1]),
                   r=[bank, binT], w=[sq])
                first = (grp == 1) or (slot == 0)
                last = (grp == 1) or (slot == 1)
                op("tensor", lambda e, sq=sq, grp=grp, first=first, last=last: e.matmul(
                    ssb[grp][:, 0:ntok], lhsT=ones[:], rhs=sq[:, 0:ntok], start=first, stop=last),
                   r=[ones, sq], w=[ssb[grp]], inc=last)
            for grp, width in ((0, 256.0), (1, 128.0)):
                if is_ctx and grp == 0:
                    continue
                op("scalar", lambda e, rs=rs, grp=grp, width=width: e.activation(
                    out=rs[:, grp, 0:ntok], in_=ssb[grp][:, 0:ntok], func=AF.Sqrt, scale=1.0 / width, bias=EPS),
                   r=[ssb[grp]], w=[rs])
                op("vector", lambda e, rs=rs, grp=grp: e.reciprocal(out=rs[:, grp, 0:ntok], in_=rs[:, grp, 0:ntok]),
                   r=[rs], w=[rs])
            if not is_ctx:
                op("vector", lambda e, rs=rs: e.tensor_scalar(
                    out=rs[:, 0, 0:ntok], in0=rs[:, 0, 0:ntok], scalar1=ATTN_SCALE, scalar2=None, op0=ALU.mult),
                   r=[rs], w=[rs])

            kp = kpr.next()
            bank = prot.next()
            proj(C_KPE, 32, bank)
            op("scalar", lambda e, kp=kp, bank=bank: e.activation(
                out=kp[:, 0, 0:ntok], in_=bank[0:32, 0:ntok], func=AF.Identity, bias=binT[0:32, 31:32]),
               r=[bank, binT], w=[kp])
            if not is_ctx:
                rop = ropr.next()
                dma("sync", rop[64:96, 0, :], ropeC_d[:, tok0:tok0 + 512], w=[rop], key=rop)
                dma("sync", rop[64:96, 1, :], ropeS_d[:, tok0:tok0 + 512], w=[rop], key=rop)
                dma("sync", rop[0:32, 0, :], ropeC_d[:, tok0:tok0 + 512], w=[rop], key=rop)
                dma("sync", rop[0:32, 1, :], ropeS_d[:, tok0:tok0 + 512], w=[rop], key=rop)
                bank2 = prot.next()
                proj(0, 32, bank2, wsrc=wksw)
                op("scalar", lambda e, kp=kp, bank2=bank2: e.activation(
                    out=kp[:, 1, 0:ntok], in_=bank2[0:32, 0:ntok], func=AF.Identity, bias=binT[0:32, 32:33]),
                   r=[bank2, binT], w=[kp])
                op("vector", lambda e, kp=kp, rop=rop: e.tensor_tensor(
                    out=kp[:, 0, :], in0=kp[:, 0, :], in1=rop[0:32, 0, :], op=ALU.mult), r=[kp, rop], w=[kp])
                op("vector", lambda e, kp=kp, rop=rop: e.tensor_tensor(
                    out=kp[:, 1, :], in0=kp[:, 1, :], in1=rop[0:32, 1, :], op=ALU.mult), r=[kp, rop], w=[kp])
                op("vector", lambda e, kp=kp: e.tensor_tensor(
                    out=kp[:, 0, :], in0=kp[:, 0, :], in1=kp[:, 1, :], op=ALU.add), r=[kp], w=[kp])
                op("vector", lambda e, rop=rop, rs=rs: e.tensor_tensor(
                    out=rop[64:96, :, :], in0=rop[64:96, :, :],
                    in1=rs[64:96, 0:1, :].to_broadcast([32, 2, 512]), op=ALU.mult), r=[rop, rs], w=[rop])

            for hd in range(H):
                ko = kor.next()
                bank = prot.next()
                op("tensor", lambda e, bank=bank, hd=hd, ql=ql: e.matmul(
                    bank[0:64, 0:ntok], lhsT=wkv[:, hd * 64:(hd + 1) * 64], rhs=ql[:, 2, 0:ntok], start=True, stop=True),
                   r=[wkv, ql], w=[bank])
                op("vector", lambda e, ko=ko, bank=bank, rs=rs: e.tensor_tensor(
                    out=ko[0:64, 0:ntok], in0=bank[0:64, 0:ntok], in1=rs[0:64, 1, 0:ntok], op=ALU.mult),
                   r=[bank, rs], w=[ko])
                dma("sync", Ks[hd, 0:64, key0:key0 + ntok], ko[0:64, 0:ntok], r=[ko], key=ko)
            kpbf = qor.next()
            op("scalar", lambda e, kpbf=kpbf, kp=kp: e.copy(out=kpbf[0:32, 0:ntok], in_=kp[:, 0, 0:ntok]), r=[kp], w=[kpbf])
            for hd in range(H):
                dma("sync", Ks[hd, 64:96, key0:key0 + ntok], kpbf[0:32, 0:ntok], r=[kpbf], key=kpbf)

            if not is_ctx:
                for hd in range(H):
                    qo = qor.next()
                    bank = prot.next()
                    bank2 = prot.next()
                    for k in range(2):
                        op("tensor", lambda e, bank=bank, hd=hd, k=k, ql=ql: e.matmul(
                            bank[0:96, :], lhsT=wuq[:, k, hd * DQ:(hd + 1) * DQ], rhs=ql[:, k, :], start=(k == 0), stop=(k == 1)),
                           r=[wuq, ql], w=[bank], inc=(k == 1))
                    for k in range(2):
                        op("tensor", lambda e, bank2=bank2, hd=hd, k=k, ql=ql: e.matmul(
                            bank2[0:96, :], lhsT=wuqsw[:, k, hd * DQ:(hd + 1) * DQ], rhs=ql[:, k, :], start=(k == 0), stop=(k == 1)),
                           r=[wuqsw, ql], w=[bank2], inc=(k == 1))
                    op("vector", lambda e, qo=qo, bank=bank, rs=rs: e.tensor_tensor(
                        out=qo[0:64, :], in0=bank[0:64, :], in1=rs[0:64, 0, :], op=ALU.mult), r=[bank, rs], w=[qo])
                    tmp = tmpr.next()
                    op("vector", lambda e, tmp=tmp, bank=bank, rop=rop: e.tensor_tensor(
                        out=tmp[64:96, :], in0=bank[64:96, :], in1=rop[64:96, 0, :], op=ALU.mult), r=[bank, rop], w=[tmp])
                    tmp2 = tmpr.next()
                    op("vector", lambda e, tmp2=tmp2, bank2=bank2, rop=rop: e.tensor_tensor(
                        out=tmp2[64:96, :], in0=bank2[64:96, :], in1=rop[64:96, 1, :], op=ALU.mult), r=[bank2, rop], w=[tmp2])
                    op("gpsimd", lambda e, qo=qo, tmp=tmp, tmp2=tmp2: e.tensor_tensor(
                        out=qo[64:96, :], in0=tmp[64:96, :], in1=tmp2[64:96, :], op=ALU.add), r=[tmp, tmp2], w=[qo])
                    dma("sync", Qs[hd, :, tok0:tok0 + 512], qo[:, :], r=[qo], key=qo)

            for i in range(nt):
                bank = prot.next()
                op("tensor", lambda e, bank=bank, i=i, ql=ql: e.matmul(
                    bank[:, :], lhsT=ql[:, 2, i * 128:(i + 1) * 128], rhs=wkv[:, 512:1024], start=True, stop=True),
                   r=[ql, wkv], w=[bank])
                bank3 = prot.next()
                op("tensor", lambda e, bank3=bank3, i=i, rs=rs: e.transpose(
                    out=bank3[:, 0:128], in_=rs[:, 1, i * 128:(i + 1) * 128], identity=ident[:]),
                   r=[rs, ident], w=[bank3])
                op("scalar", lambda e, rcol=rcol, bank3=bank3, i=i: e.copy(out=rcol[:, i:i + 1], in_=bank3[:, 0:1]),
                   r=[bank3], w=[rcol])
                vo = vor.next()
                op("vector", lambda e, vo=vo, bank=bank, rcol=rcol, i=i: e.tensor_scalar(
                    out=vo[:], in0=bank[:], scalar1=rcol[:, i:i + 1], scalar2=None, op0=ALU.mult), r=[bank, rcol], w=[vo])
                dma("sync", Vs[key0 + i * 128: key0 + (i + 1) * 128, :], vo[:], r=[vo], key=vo)

            if is_ctx:
                return
            for j in range(12):
                bank = prot.next()
                proj(C_HY + j * 128, 128, bank)
                op("scalar", lambda e, j=j: e.copy(out=ptall[:, j, 0:2], in_=ptall[:, j, 512:514]), r=[ptall], w=[ptall])
                op("scalar", lambda e, j=j, bank=bank: e.activation(
                    out=ptall[:, j, 2:514], in_=bank[:, :], func=AF.Identity, bias=binT[:, 3 + j:4 + j]),
                   r=[bank, binT], w=[ptall])
                ub = ubr.next()
                conv_tile(ub, j)
                dma("sync", Us[j * 128:(j + 1) * 128, tok0:tok0 + 512], ub[:], r=[ub], key=ub)
            for j in range(16):
                bank = prot.next()
                proj(C_GATE + j * 128, 128, bank)
                gb = gbr.next()
                op("scalar", lambda e, gb=gb, bank=bank, j=j: e.activation(
                    out=gb[:], in_=bank[:], func=AF.Sigmoid, bias=binT[:, 15 + j:16 + j]), r=[bank, binT], w=[gb])
                dma("sync", Gs[j * 128:(j + 1) * 128, tok0:tok0 + 512], gb[:], r=[gb], key=gb)

        cvr = kb.rot("cv", 2, [128, 512], F32)

        def conv_tile(ub, j, n=512, src0=0):
            cv = cvr.next()
            op("vector", lambda e, cv=cv, j=j: e.tensor_scalar(
                out=cv[:, 0:n], in0=ptall[:, j, src0:src0 + n], scalar1=hcw[:, 3 * j:3 * j + 1], scalar2=hcb[:, j:j + 1],
                op0=ALU.mult, op1=ALU.add), r=[ptall, hcw, hcb], w=[cv])
            op("vector", lambda e, cv=cv, j=j: e.scalar_tensor_tensor(
                out=cv[:, 0:n], in0=ptall[:, j, src0 + 1:src0 + 1 + n], scalar=hcw[:, 3 * j + 1:3 * j + 2], in1=cv[:, 0:n],
                op0=ALU.mult, op1=ALU.add), r=[ptall, hcw, cv], w=[cv])
            op("vector", lambda e, cv=cv, j=j, ub=ub: e.scalar_tensor_tensor(
                out=ub[:, 0:n], in0=ptall[:, j, src0 + 2:src0 + 2 + n], scalar=hcw[:, 3 * j + 2:3 * j + 3], in1=cv[:, 0:n],
                op0=ALU.mult, op1=ALU.add), r=[ptall, hcw, cv], w=[ub])

        def bank_index(bank):
            return pb.index(bank)

        nchunks = SEQ // 512
        cgens = [proj_chunk(0, 256, True)] + [proj_chunk(ci * 512, 512, False) for ci in range(nchunks)]
        next(cgens[0])
        for ci_ in range(len(cgens)):
            if ci_ + 1 < len(cgens):
                next(cgens[ci_ + 1])
            for _ in cgens[ci_]:
                pass
        for j in range(12):
            op("scalar", lambda e, j=j: e.copy(out=ptall[:, j, 0:2], in_=ptall[:, j, 512:514]), r=[ptall], w=[ptall])
            op("vector", lambda e, j=j: e.memset(ptall[:, j, 2:3], 0.0), w=[ptall])
            ub = ubr.next()
            conv_tile(ub, j, n=1, src0=0)
            dma("sync", Us[j * 128:(j + 1) * 128, SEQ:SEQ + 1], ub[:, 0:1], r=[ub], key=ub, allow_slow_non_contiguous=True)
        kb.end_phase()


    EWg = kb.scratch("EWg", [NE, 128, 8 * EFF], BF16)
    EWu = kb.scratch("EWu", [NE, 128, 8 * EFF], BF16)
    EWd = kb.scratch("EWd", [NE, 128, 2 * D], BF16)
    ne_lim = lim.get("experts", NE)
    if "moe" in phases:
        weg_d = kb.inp("w_exp_gate", [NE, D, EFF]); weu_d = kb.inp("w_exp_up", [NE, D, EFF]); wed_d = kb.inp("w_exp_down", [NE, EFF, D])

    def convert_expert_weights():
        cvb = [Buf("cv%d" % i) for i in range(4)]
        kb.phase_bufs.extend(cvb)
        for e_ in range(ne_lim):
            dma("gpsimd", EWg[e_].rearrange("p (k f) -> p k f", k=8), weg_d[e_].rearrange("(k p) f -> p k f", p=128), key=cvb[e_ % 4])
            dma("gpsimd", EWu[e_].rearrange("p (k f) -> p k f", k=8), weu_d[e_].rearrange("(k p) f -> p k f", p=128), key=cvb[e_ % 4])
            dma("gpsimd", EWd[e_].rearrange("p (k d) -> p k d", k=2), wed_d[e_].rearrange("(k p) d -> p k d", p=128), key=cvb[e_ % 4])
    converted = [False]

    ATs = kb.scratch("ATs", [512, SEQ], BF16)
    if "attn" in phases:
        kb.begin_phase()
        Ktr = kb.rot("Kt", 2, [96, NKEY], BF16)
        Vtr = kb.rot("Vt", 2, [128, 66, 128], BF16)
        Qtr = kb.rot("Qt", 2, [96, SEQ], BF16)
        ptr_ = kb.rot("pt", 4, [128, 1024], BF16)
        osr = kb.rot("os", 2, [64, 512], F32)
        rdr = kb.rot("rd", 2, [128, 512], F32)
        aor = kb.rot("ao", 2, [64, 512], BF16)
        for vt in Vtr.bufs:
            op("vector", lambda e, vt=vt: e.memset(vt[:, :, 64:128], 1.0), w=[vt])
        if "moe" in phases:
            convert_expert_weights()
            converted[0] = True
        Vs_v = Vs.rearrange("(kt p) c -> p kt c", p=128)

        def load_head(hd):
            kt_, vt_, qt_ = Ktr.next(), Vtr.next(), Qtr.next()
            dma("sync", kt_[:], Ks[hd], w=[kt_], key=kt_)
            dma("sync", vt_[:, :, 0:64], Vs_v[:, :, hd * 64:(hd + 1) * 64], w=[vt_], key=vt_)
            dma("sync", qt_[:], Qs[hd], w=[qt_], key=qt_)
            return kt_, vt_, qt_

        spair = [(pb[0], pb[1]), (pb[2], pb[3]), (pb[4], pb[5])]
        pobanks = [pb[6], pb[6]]
        bcbanks = [pb[7], pb[7]]
        items = []
        for hd in range(H):
            for qc in range(SEQ // 512):
                for pi in range(33):
                    items.append((hd, qc, pi))
        heads = {0: load_head(0)}
        state = {}
        pend = []

        def emit_S(i):
            hd, qc, pi = items[i]
            if qc == 1 and pi == 0 and hd + 1 < H:
                heads[hd + 1] = load_head(hd + 1)
            kt_, vt_, qt_ = heads[hd]
            b0, b1 = spair[i % 3]
            for j, bk in enumerate((b0, b1)):
                ktile = pi * 2 + j
                op("tensor", lambda e, bk=bk, kt_=kt_, qt_=qt_, ktile=ktile, qc=qc: e.matmul(
                    bk[:, :], lhsT=kt_[:, ktile * 128:(ktile + 1) * 128], rhs=qt_[:, qc * 512:(qc + 1) * 512],
                    start=True, stop=True), r=[kt_, qt_], w=[bk])
            pt = ptr_.next()
            state[i] = pt
            bi = pb.index(b0)
            op("scalar", lambda e, pt=pt, bi=bi: e.activation(out=pt[:, :], in_=kb.P[:, bi * 512:bi * 512 + 1024], func=AF.Exp),
               r=[b0, b1], w=[pt])

        def emit_PV(i):
            hd, qc, pi = items[i]
            kt_, vt_, qt_ = heads[hd]
            pt = state.pop(i)
            g = hd * (SEQ // 512) + qc
            po = pobanks[g % 2]
            for j in range(2):
                ktile = pi * 2 + j
                op("tensor", lambda e, po=po, vt_=vt_, pt=pt, ktile=ktile, j=j: e.matmul(
                    po[:, :], lhsT=vt_[:, ktile, :], rhs=pt[:, j * 512:(j + 1) * 512],
                    start=(ktile == 0), stop=(ktile == 65)), r=[vt_, pt], w=[po], inc=(j == 1))
            if pi == 32:
                osb, rd, bc = osr.next(), rdr.next(), bcbanks[g % 2]
                op("scalar", lambda e, osb=osb, po=po: e.copy(out=osb[:, :], in_=po[0:64, :]), r=[po], w=[osb])
                op("vector", lambda e, rd=rd, po=po: e.reciprocal(out=rd[64:65, :], in_=po[64:65, :]), r=[po], w=[rd])

                def fin(hd=hd, qc=qc, osb=osb, rd=rd, bc=bc):
                    op("tensor", lambda e: e.matmul(bc[0:64, :], lhsT=ones[64:65, 0:64], rhs=rd[64:65, :], start=True, stop=True),
                       r=[ones, rd], w=[bc])
                    ao = aor.next()
                    op("vector", lambda e, ao=ao: e.tensor_tensor(out=ao[:, :], in0=osb[:, :], in1=bc[0:64, :], op=ALU.mult),
                       r=[osb, bc], w=[ao])
                    dma("sync", ATs[hd * 64:(hd + 1) * 64, qc * 512:(qc + 1) * 512], ao[:, :], r=[ao], key=ao)
                pend.append((i + 3, fin))

        n = len(items)
        DEPTH = 2
        for i in range(n + DEPTH):
            if i < n:
                emit_S(i)
            if i >= DEPTH:
                emit_PV(i - DEPTH)
            while pend and pend[0][0] <= i:
                pend.pop(0)[1]()
        while pend:
            pend.pop(0)[1]()
        kb.end_phase()


    FTs = kb.scratch("FTs", [2048, SEQ], BF16)
    KFs = kb.scratch("KFs", [2, 128, 2, HYW, 128], BF16)
    HYs = kb.scratch("HYs", [64, HYW, 128], BF16)
    need_fft = ("filt" in phases) or ("hyena" in phases)
    if need_fft:
        dC_d = kb.inp("dftC", [128, 128]); dS_d = kb.inp("dftS", [128, 128])
        twC_d = kb.inp("twC", [128, 128]); twS_d = kb.inp("twS", [128, 128])
        tC = kb.sb("tC", [128, 128], BF16, persist=True)
        tS = kb.sb("tS", [128, 128], BF16, persist=True)
        tnS = kb.sb("tnS", [128, 128], BF16, persist=True)
        tCS = kb.sb("tCS", [128, 256], BF16, persist=True)
        tSnC = kb.sb("tSnC", [128, 256], BF16, persist=True)
        tCShi = kb.sb("tCShi", [64, 256], BF16, persist=True)
        twC = kb.sb("twC", [128, 128], F32, persist=True)
        twS = kb.sb("twS", [128, 128], F32, persist=True)
        kb.begin_phase()
        stgC = kb.sb("stgC", [128, 128], F32); stgS = kb.sb("stgS", [128, 128], F32)
        stgH = kb.sb("stgH", [64, 256], F32)
        dma("sync", stgC[:], dC_d, w=[stgC], key=stgC)
        dma("sync", stgS[:], dS_d, w=[stgS], key=stgS)
        dma("sync", stgH[:, 0:128], dC_d[64:128, :], w=[stgH], key=stgH)
        dma("sync", stgH[:, 128:256], dS_d[64:128, :], w=[stgH], key=stgH)
        dma("sync", twC[:], twC_d, w=[twC], key=twC)
        dma("sync", twS[:], twS_d, w=[twS], key=twS)
        op("vector", lambda e: e.tensor_copy(out=tC[:], in_=stgC[:]), r=[stgC], w=[tC])
        op("vector", lambda e: e.tensor_copy(out=tS[:], in_=stgS[:]), r=[stgS], w=[tS])
        op("vector", lambda e: e.tensor_scalar(out=tnS[:], in0=stgS[:], scalar1=-1.0, scalar2=None, op0=ALU.mult), r=[stgS], w=[tnS])
        op("vector", lambda e: e.tensor_copy(out=tCS[:, 0:128], in_=stgC[:]), r=[stgC], w=[tCS])
        op("vector", lambda e: e.tensor_copy(out=tCS[:, 128:256], in_=stgS[:]), r=[stgS], w=[tCS])
        op("vector", lambda e: e.tensor_copy(out=tSnC[:, 0:128], in_=stgS[:]), r=[stgS], w=[tSnC])
        op("vector", lambda e: e.tensor_scalar(out=tSnC[:, 128:256], in0=stgC[:], scalar1=-1.0, scalar2=None, op0=ALU.mult), r=[stgC], w=[tSnC])
        op("vector", lambda e: e.tensor_copy(out=tCShi[:], in_=stgH[:]), r=[stgH], w=[tCShi])
        kb.end_phase()

    pairs = Rot([(pb[0], pb[1]), (pb[2], pb[3]), (pb[4], pb[5]), (pb[6], pb[7])])

    def pair_ap(pr):
        i0 = pb.index(pr[0])
        return kb.P[:, i0 * 512:i0 * 512 + 1024]

    def interleave(gens, width):
        gens = iter(gens)
        active = []
        done = False
        while True:
            while not done and len(active) < width:
                try:
                    active.append(next(gens))
                except StopIteration:
                    done = True
            if not active:
                break
            for g_ in list(active):
                try:
                    next(g_)
                except StopIteration:
                    active.remove(g_)

    class FFT:
        def __init__(self):
            self.tar = kb.rot("ta", 3, [128, 1024], F32)
            self.tbr = kb.rot("tb", 3, [128, 1024], F32)
            self.brr = kb.rot("br", 6, [128, 4, 128], BF16)
            self.bir = kb.rot("bi", 6, [128, 4, 128], BF16)

        def cmul(self, pr, lay, tcv, tsv, tabs, o_re, o_im, obufs_re, obufs_im):
            pa = pair_ap(pr)
            ta, tb = self.tar.next(), self.tbr.next()
            if lay == 1:
                pv = pa.rearrange("p (c t k) -> p c t k", c=4, t=2)
                tav = ta[:].rearrange("p (c t k) -> p c t k", c=4, t=2)
                tbv = tb[:].rearrange("p (c t k) -> p c t k", c=4, t=2)
                a0, a1 = tav[:, :, 0, :], tav[:, :, 1, :]
                b0, b1 = tbv[:, :, 0, :], tbv[:, :, 1, :]
            else:
                pv = pa.rearrange("p (t c k) -> p t c k", t=2, c=4)
                tav = ta[:].rearrange("p (t c k) -> p t c k", t=2, c=4)
                tbv = tb[:].rearrange("p (t c k) -> p t c k", t=2, c=4)
                a0, a1 = tav[:, 0], tav[:, 1]
                b0, b1 = tbv[:, 0], tbv[:, 1]
            op("vector", lambda e: e.tensor_tensor(out=tav, in0=pv, in1=tcv, op=ALU.mult), r=[pr[0], pr[1]] + tabs, w=[ta])
            op("vector", lambda e: e.tensor_tensor(out=tbv, in0=pv, in1=tsv, op=ALU.mult), r=[pr[0], pr[1]] + tabs, w=[tb])
            op("gpsimd", lambda e: e.tensor_tensor(out=o_re, in0=a0, in1=b1, op=ALU.subtract), r=[ta, tb], w=obufs_re)
            op("gpsimd", lambda e: e.tensor_tensor(out=o_im, in0=b0, in1=a1, op=ALU.add), r=[ta, tb], w=obufs_im)

        def forward(self, srcs):
            pr = pairs.next()
            pa = pair_ap(pr)
            for c in range(4):
                for si, (sbuf, sap, tbuf, tap) in enumerate(srcs):
                    last = si == len(srcs) - 1
                    op("tensor", lambda e, c=c, sap=sap, tap=tap, si=si, last=last: e.matmul(
                        pa[:, c * 256:(c + 1) * 256], lhsT=sap[:, c, :], rhs=tap, start=(si == 0), stop=last),
                       r=[sbuf, tbuf], w=[pr[0], pr[1]], inc=(last and c == 3))
            yield
            br, bi = self.brr.next(), self.bir.next()
            tcv = twC[:].unsqueeze(1).unsqueeze(1).to_broadcast([128, 4, 2, 128])
            tsv = twS[:].unsqueeze(1).unsqueeze(1).to_broadcast([128, 4, 2, 128])
            self.cmul(pr, 1, tcv, tsv, [twC, twS], br[:], bi[:], [br], [bi])
            yield
            pr2 = pairs.next()
            brv = br[:].rearrange("p c k -> p (c k)")
            biv = bi[:].rearrange("p c k -> p (c k)")
            for (bank, la, lb) in ((pr2[0], tC, tnS), (pr2[1], tS, tC)):
                op("tensor", lambda e, bank=bank, la=la: e.matmul(bank[:, :], lhsT=la[:], rhs=brv, start=True, stop=False),
                   r=[la, br], w=[bank], inc=False)
                op("tensor", lambda e, bank=bank, lb=lb: e.matmul(bank[:, :], lhsT=lb[:], rhs=biv, start=False, stop=True),
                   r=[lb, bi], w=[bank])
            yield
            return pr2

        def inverse(self, yre, yim):
            pr = pairs.next()
            pa = pair_ap(pr)
            for c in range(4):
                op("tensor", lambda e, c=c: e.matmul(pa[:, c * 256:(c + 1) * 256], lhsT=yre[:, c, :], rhs=tCS[:], start=True, stop=False),
                   r=[yre, tCS], w=[pr[0], pr[1]], inc=False)
                op("tensor", lambda e, c=c: e.matmul(pa[:, c * 256:(c + 1) * 256], lhsT=yim[:, c, :], rhs=tSnC[:], start=False, stop=True),
                   r=[yim, tSnC], w=[pr[0], pr[1]], inc=(c == 3))
            yield
            hr_, hi_ = self.brr.next(), self.bir.next()
            tcv = twC[:].unsqueeze(1).unsqueeze(1).to_broadcast([128, 4, 2, 128])
            tsv = twS[:].unsqueeze(1).unsqueeze(1).to_broadcast([128, 4, 2, 128])
            self.cmul(pr, 1, tcv, tsv, [twC, twS], hr_[:], hi_[:], [hr_], [hi_])
            yield
            pr3 = pairs.next()
            bank = pr3[0]
            op("tensor", lambda e: e.matmul(bank[0:64, :], lhsT=tC[:, 0:64], rhs=hr_[:].rearrange("p c k -> p (c k)"), start=True, stop=False),
               r=[tC, hr_], w=[bank], inc=False)
            op("tensor", lambda e: e.matmul(bank[0:64, :], lhsT=tnS[:, 0:64], rhs=hi_[:].rearrange("p c k -> p (c k)"), start=False, stop=True),
               r=[tnS, hi_], w=[bank])
            yield
            return bank

    if "filt" in phases:
        embF_d = kb.inp("embF", [17, SEQ]); embB_d = kb.inp("embB", [17, SEQ])
        w1_d = kb.inp("hf_w1", [17, 64]); w2_d = kb.inp("hf_w2", [64, 64]); w3_d = kb.inp("hf_w3", [64, 2048])
        hfv_d = kb.inp("hf_vecs", [64, 3])
        trF_d = kb.inp("trowF", [128, SEQ]); trB_d = kb.inp("trowB", [128, SEQ])
        ndl_d = kb.inp("negdelta", [128, 4])
        skb_d = kb.inp("skip_b", [128, 2, HYW])
        kb.begin_phase()
        w1 = kb.sb("w1", [17, 64], F32); w2 = kb.sb("w2", [64, 64], F32); w3 = kb.sb("w3", [64, 2048], F32)
        hfv = kb.sb("hfv", [64, 8], F32)
        ndl = kb.sb("ndl", [128, 4], F32)
        h2T = [kb.sb("h2TF", [64, SEQ], F32), kb.sb("h2TB", [64, SEQ], F32)]
        embr = kb.rot("emb", 2, [17, 512], F32)
        ur = kb.rot("u", 2, [64, 512], F32)
        u2r = kb.rot("u2", 2, [64, 512], F32)
        h1r = kb.rot("h1", 2, [64, 512], F32)
        l1p = kb.sb("l1p", [128, 16, 16], F32)
        dma("sync", w1[:], w1_d, w=[w1], key=w1)
        dma("sync", w2[:], w2_d, w=[w2], key=w2)
        dma("sync", w3[:], w3_d, w=[w3], key=w3)
        dma("sync", hfv[:, 0:3], hfv_d, w=[hfv], key=hfv)
        dma("sync", ndl[:], ndl_d, w=[ndl], key=ndl)
        TWO_PI = 2.0 * math.pi
        op("vector", lambda e: e.tensor_scalar(out=hfv[:, 3:4], in0=hfv[:, 2:3], scalar1=1.0 / TWO_PI, scalar2=None, op0=ALU.mult), r=[hfv], w=[hfv])
        op("vector", lambda e: e.tensor_tensor(out=hfv[:, 4:5], in0=hfv[:, 3:4], in1=hfv[:, 0:1], op=ALU.mult), r=[hfv], w=[hfv])
        op("vector", lambda e: e.tensor_tensor(out=hfv[:, 5:6], in0=hfv[:, 3:4], in1=hfv[:, 1:2], op=ALU.mult), r=[hfv], w=[hfv])

        def sin_layer(bank, bcol, dst, dbuf):
            u = ur.next()
            op("scalar", lambda e, u=u: e.activation(out=u[:], in_=bank[0:64, :], func=AF.Identity, scale=hfv[:, 3:4], bias=hfv[:, bcol:bcol + 1]),
               r=[bank, hfv], w=[u])
            u2 = u2r.next()
            for it in range(2):
                op("vector", lambda e, u=u, u2=u2: e.scalar_tensor_tensor(out=u2[:], in0=u[:], scalar=0.5, in1=u[:], op0=ALU.is_gt, op1=ALU.subtract),
                   r=[u], w=[u2])
                op("vector", lambda e, u=u, u2=u2: e.scalar_tensor_tensor(out=u[:], in0=u2[:], scalar=0.5, in1=u2[:], op0=ALU.is_gt, op1=ALU.subtract),
                   r=[u2], w=[u])
            op("scalar", lambda e, u=u: e.activation(out=dst, in_=u[:], func=AF.Sin, scale=6.283185), r=[u], w=[dbuf])

        for di, emb_d in enumerate((embF_d, embB_d)):
            for pc in range(16):
                em = embr.next()
                dma("sync", em[:], emb_d[:, pc * 512:(pc + 1) * 512], w=[em], key=em)
                bank = prot.next()
                op("tensor", lambda e, bank=bank, em=em: e.matmul(bank[0:64, :], lhsT=w1[:], rhs=em[:], start=True, stop=True), r=[w1, em], w=[bank])
                h1 = h1r.next()
                sin_layer(bank, 4, h1[:], h1)
                bank2 = prot.next()
                op("tensor", lambda e, bank2=bank2, h1=h1: e.matmul(bank2[0:64, :], lhsT=w2[:], rhs=h1[:], start=True, stop=True), r=[w2, h1], w=[bank2])
                sin_layer(bank2, 5, h2T[di][:, pc * 512:(pc + 1) * 512], h2T[di])

        trr = kb.rot("tr", 2, [128, 512], F32)
        winr = kb.rot("win", 2, [128, 512], F32)
        kwr = kb.rot("kw", 2, [128, 512], F32)
        kbr = kb.rot("kbf", 3, [128, 512], BF16)
        op("vector", lambda e: e.memset(l1p[:], 0.0), w=[l1p])
        for di, tr_d in enumerate((trF_d, trB_d)):
            for pc in range(16):
                tr = trr.next()
                dma("sync", tr[:], tr_d[:, pc * 512:(pc + 1) * 512], w=[tr], key=tr)
                for cq in range(4):
                    win = winr.next()
                    op("scalar", lambda e, win=win, tr=tr, cq=cq: e.activation(out=win[:], in_=tr[:], func=AF.Exp, scale=ndl[:, cq:cq + 1]),
                       r=[tr, ndl], w=[win])
                    for o in range(2):
                        ct = (2 * o + di) * 4 + cq
                        bank = prot.next()
                        op("tensor", lambda e, bank=bank, ct=ct, di=di, pc=pc: e.matmul(
                            bank[:, :], lhsT=w3[:, ct * 128:(ct + 1) * 128], rhs=h2T[di][:, pc * 512:(pc + 1) * 512], start=True, stop=True),
                           r=[w3, h2T[di]], w=[bank])
                        kw = kwr.next()
                        op("vector", lambda e, kw=kw, bank=bank, win=win: e.tensor_tensor(out=kw[:], in0=bank[:, :], in1=win[:], op=ALU.mult),
                           r=[bank, win], w=[kw])
                        op("vector", lambda e, kw=kw, ct=ct, pc=pc: e.tensor_reduce(
                            out=l1p[:, ct, pc:pc + 1], in_=kw[:], axis=AX.X, op=ALU.add, apply_absolute_value=True), r=[kw], w=[l1p])
                        kbf = kbr.next()
                        op("gpsimd", lambda e, kbf=kbf, kw=kw: e.tensor_copy(out=kbf[:], in_=kw[:]), r=[kw], w=[kbf])
                        dma("sync", FTs[ct * 128:(ct + 1) * 128, pc * 512:(pc + 1) * 512], kbf[:], r=[kbf], key=kbf)
        l1c = kb.sb("l1c", [128, 16], F32)
        rnc = kb.sb("rnc", [128, 8], F32)
        rnb = kb.sb("rnb", [128, 2, HYW], F32)
        skb = kb.sb("skb", [128, 2, HYW], F32)
        dg = kb.rot("dg", 2, [128, 128], F32)
        dma("sync", skb[:], skb_d, w=[skb], key=skb)
        op("vector", lambda e: e.tensor_scalar(out=skb[:], in0=skb[:], scalar1=1.0 / NFFT, scalar2=None, op0=ALU.mult), r=[skb], w=[skb])
        op("vector", lambda e: e.tensor_reduce(out=l1c[:], in_=l1p[:], axis=AX.X, op=ALU.add), r=[l1p], w=[l1c])
        for o in range(2):
            op("vector", lambda e, o=o: e.tensor_tensor(out=rnc[:, o * 4:(o + 1) * 4], in0=l1c[:, (2 * o) * 4:(2 * o) * 4 + 4],
                                                         in1=l1c[:, (2 * o + 1) * 4:(2 * o + 1) * 4 + 4], op=ALU.add), r=[l1c], w=[rnc])
        op("vector", lambda e: e.tensor_scalar(out=rnc[:], in0=rnc[:], scalar1=1e-6, scalar2=float(NFFT), op0=ALU.add, op1=ALU.mult), r=[rnc], w=[rnc])
        op("vector", lambda e: e.reciprocal(out=rnc[:], in_=rnc[:]), r=[rnc], w=[rnc])
        for o in range(2):
            for cq in range(4):
                d_ = dg.next()
                op("vector", lambda e, d_=d_, o=o, cq=cq: e.tensor_scalar(out=d_[:], in0=ident[:], scalar1=rnc[:, o * 4 + cq:o * 4 + cq + 1],
                                                                         scalar2=None, op0=ALU.mult), r=[ident, rnc], w=[d_])
                bank = prot.next()
                op("tensor", lambda e, bank=bank, d_=d_: e.matmul(bank[:, 0:128], lhsT=ones[:], rhs=d_[:], start=True, stop=True), r=[ones, d_], w=[bank])
                op("scalar", lambda e, bank=bank, o=o, cq=cq: e.copy(out=rnb[:, o, cq * 128:(cq + 1) * 128], in_=bank[:, 0:128]), r=[bank], w=[rnb])
        RNs = kb.scratch("RNs", [2, 128, 2 * HYW], F32)
        dma("sync", RNs[0], rnb[:].rearrange("p o c -> p (o c)"), r=[rnb], key=rnb)
        dma("sync", RNs[1], skb[:].rearrange("p o c -> p (o c)"), r=[skb], key=skb)
        kb.end_phase()
        kb.begin_phase()
        rnb = kb.sb("rnb2", [128, 2, HYW], F32)
        skb = kb.sb("skb2", [128, 2, HYW], F32)
        dma("sync", rnb[:].rearrange("p o c -> p (o c)"), RNs[0], w=[rnb], key=rnb)
        dma("sync", skb[:].rearrange("p o c -> p (o c)"), RNs[1], w=[skb], key=skb)
        fft = FFT()
        ffr = kb.rot("ff", 3, [64, 16, 128], BF16)
        fbr = kb.rot("fb", 3, [64, 16, 128], BF16)
        t32r = kb.rot("t32", 3, [128, 2, 4, 128], F32)
        kfr = kb.rot("kf", 2, [128, 2, 16, 128], BF16)
        def filt_block(o, cb):
            ff, fb = ffr.next(), fbr.next()
            r0 = (2 * o) * 512 + cb * 16
            r1 = (2 * o + 1) * 512 + cb * 16
            dma("sync", ff[:], FTs[r0:r0 + 16, :].rearrange("c (a b) -> a c b", b=128), w=[ff], key=ff)
            dma("sync", fb[:], FTs[r1:r1 + 16, :].rearrange("c (a b) -> a c b", b=128), w=[fb], key=fb)
            kf = kfr.next()

            def chain(g):
                pr2 = yield from fft.forward([(ff, ff[:, g * 4:(g + 1) * 4, :], tCS, tCS[0:64, :]),
                                              (fb, fb[:, g * 4:(g + 1) * 4, :], tCShi, tCShi[0:64, :])])
                c0 = cb * 16 + g * 4
                t32 = t32r.next()
                pv = pair_ap(pr2).rearrange("p (t c k) -> p t c k", t=2, c=4)
                op("vector", lambda e: e.tensor_tensor(
                    out=t32[:], in0=pv, in1=rnb[:, o, c0:c0 + 4].unsqueeze(1).unsqueeze(3).to_broadcast([128, 2, 4, 128]), op=ALU.mult),
                   r=[pr2[0], pr2[1], rnb], w=[t32])
                op("gpsimd", lambda e: e.tensor_tensor(
                    out=kf[:, 0, g * 4:(g + 1) * 4, :], in0=t32[:, 0], in1=skb[:, o, c0:c0 + 4].unsqueeze(2).to_broadcast([128, 4, 128]), op=ALU.add),
                   r=[t32, skb], w=[kf])
                op("scalar", lambda e: e.copy(out=kf[:, 1, g * 4:(g + 1) * 4, :], in_=t32[:, 1]), r=[t32], w=[kf])
                yield
            left = [4]

            def wrapped(g):
                yield from chain(g)
                left[0] -= 1
                if left[0] == 0:
                    dma("sync", KFs[o, :, :, cb * 16:(cb + 1) * 16, :], kf[:], r=[kf], key=kf)
            return [wrapped(g) for g in range(4)]

        WIDTH = lim.get("fftw", 4)

        def all_fchains():
            for o in range(2):
                for cb in range(HYW // 16):
                    for c_ in filt_block(o, cb):
                        yield c_
        interleave(all_fchains(), WIDTH)
        kb.end_phase()

    if "hyena" in phases:
        kb.begin_phase()
        fft = FFT()
        vr = kb.rot("v", 3, [64, 16, 128], BF16)
        x1r = kb.rot("x1", 3, [64, 16, 128], BF16)
        x2r = kb.rot("x2", 3, [64, 16, 128], BF16)
        k0r = kb.rot("k0", 2, [128, 2, 16, 128], BF16)
        k1r = kb.rot("k1", 2, [128, 2, 16, 128], BF16)
        hyr = kb.rot("hy", 3, [64, 16, 128], BF16)
        zr = kb.rot("z", 4, [64, 4, 128], BF16)
        yrr = kb.rot("yr", 4, [128, 4, 128], BF16)
        yir = kb.rot("yi", 4, [128, 4, 128], BF16)

        def ld(buf, row0):
            dma("sync", buf[:], Us[row0:row0 + 16, 1:SEQ + 1].rearrange("c (a b) -> a c b", b=128), w=[buf], key=buf)

        WIDTH = lim.get("fftw", 4)

        def conv(src_buf, src_ap, kt, g):
            pr2 = yield from fft.forward([(src_buf, src_ap, tCS, tCS[0:64, :])])
            yr_, yi_ = yrr.next(), yir.next()
            kre = kt[:, 0, g * 4:(g + 1) * 4, :].unsqueeze(1).to_broadcast([128, 2, 4, 128])
            kim = kt[:, 1, g * 4:(g + 1) * 4, :].unsqueeze(1).to_broadcast([128, 2, 4, 128])
            fft.cmul(pr2, 2, kre, kim, [kt], yr_[:], yi_[:], [yr_], [yi_])
            yield
            bank = yield from fft.inverse(yr_, yi_)
            return bank

        def hy_block(cb):
            v, x1, x2 = vr.next(), x1r.next(), x2r.next()
            k0, k1 = k0r.next(), k1r.next()
            ld(v, cb * 16); ld(x1, 512 + cb * 16); ld(x2, 1024 + cb * 16)
            dma("sync", k0[:], KFs[0, :, :, cb * 16:(cb + 1) * 16, :], w=[k0], key=k0)
            dma("sync", k1[:], KFs[1, :, :, cb * 16:(cb + 1) * 16, :], w=[k1], key=k1)
            hy = hyr.next()

            def chain(g):
                bank = yield from conv(v, v[:, g * 4:(g + 1) * 4, :], k0, g)
                z = zr.next()
                op("vector", lambda e: e.tensor_tensor(
                    out=z[:].rearrange("p c k -> p (c k)"), in0=bank[0:64, :], in1=x1[:, g * 4:(g + 1) * 4, :].rearrange("p c k -> p (c k)"), op=ALU.mult),
                   r=[bank, x1], w=[z])
                yield
                bank2 = yield from conv(z, z[:, :, :], k1, g)
                op("vector", lambda e: e.tensor_tensor(
                    out=hy[:, g * 4:(g + 1) * 4, :].rearrange("p c k -> p (c k)"), in0=bank2[0:64, :],
                    in1=x2[:, g * 4:(g + 1) * 4, :].rearrange("p c k -> p (c k)"), op=ALU.mult), r=[bank2, x2], w=[hy])
                yield
            left = [4]

            def wrapped(g):
                yield from chain(g)
                left[0] -= 1
                if left[0] == 0:
                    dma("sync", HYs[:, cb * 16:(cb + 1) * 16, :], hy[:], r=[hy], key=hy)
            return [wrapped(g) for g in range(4)]

        def all_chains():
            for cb in range(HYW // 16):
                for c_ in hy_block(cb):
                    yield c_
        interleave(all_chains(), WIDTH)
        kb.end_phase()


    XMs = kb.scratch("XMs", [SEQ, D], F32)
    H2Ts = kb.scratch("H2Ts", [D, SEQ], BF16)
    WTs = kb.scratch("WTs", [2, NE, SEQ], BF16)
    if "merge" in phases:
        wba_d = kb.inp("w_ba", [512, D]); wbh_d = kb.inp("w_bh", [512, D]); wout_d = kb.inp("w_out", [D, D])
        wr_d = kb.inp("w_router", [D, NE]); rb_d = kb.inp("rbias_b", [128, NE])
        kb.begin_phase()
        wba = kb.sb("wba", [128, 4, D], BF16); wbh = kb.sb("wbh", [128, 4, D], BF16); wout = kb.sb("wout", [128, 8, D], BF16)
        wr = kb.sb("wr", [128, 8, NE], F32); rbb = kb.sb("rbb", [128, NE], F32)
        G2 = kb.sb("G2", [128, D], F32); A2 = kb.sb("A2", [128, D], F32); B2 = kb.sb("B2", [128, D], F32)
        dma("gpsimd", wba[:], wba_d.rearrange("(k p) n -> p k n", p=128), w=[wba], key=wba)
        dma("gpsimd", wbh[:], wbh_d.rearrange("(k p) n -> p k n", p=128), w=[wbh], key=wbh)
        for k in range(8):
            dma("gpsimd", wout[:, k, :], wout_d[k * 128:(k + 1) * 128, :], w=[wout], key=wout)
        dma("sync", wr[:], wr_d.rearrange("(k p) n -> p k n", p=128), w=[wr], key=wr)
        dma("sync", rbb[:], rb_d, w=[rbb], key=rbb)
        for t_, i_ in ((G2, 2), (A2, 4), (B2, 3)):
            dma("sync", t_[:], MODS[i_], w=[t_], key=t_)
        atr = kb.rot("at", 2, [128, 4, 512], BF16)
        hytr = kb.rot("hyt", 2, [128, 4, 512], BF16)
        gar = kb.rot("ga", 2, [128, 16, 512], BF16)
        t1r = kb.rot("t1", 2, [128, 512], F32)
        t2r = kb.rot("t2", 2, [128, 512], F32)
        yTr = kb.rot("yT", 2, [128, 8, 512], BF16)
        xr = kb.rot("x", 3, [128, D], F32)
        tmr = kb.rot("tm", 3, [128, D], F32)
        xmr = kb.rot("xm", 3, [128, D], F32)
        jkr = kb.rot("jk", 1, [128, D], BF16)
        sttr = kb.rot("stt", 4, [128, 4], F32)
        h2r = kb.rot("h2", 3, [128, D], F32)
        h2Tr = kb.rot("h2T", 3, [128, 8, 128], F32)
        h2Tbr = kb.rot("h2Tb", 2, [128, 8, 128], BF16)
        rtr = kb.rot("rt", 4, [128, 8, NE], F32)
        smr = kb.rot("sm", 4, [128, 64], F32)
        wtr = kb.rot("wt", 2, [64, 2, 128], BF16)
        wlr = kb.rot("wl", 2, [64, 128], F32)
        HY_v = HYs.rearrange("a c b -> c a b")
        BIG = 1.0e9

        def merge_chunk(ci):
            t0 = ci * 512
            at, hyt, ga = atr.next(), hytr.next(), gar.next()
            dma("sync", at[:], ATs[:, t0:t0 + 512].rearrange("(k p) t -> p k t", p=128), w=[at], key=at)
            for k in range(4):
                dma("sync", hyt[:, k, :].rearrange("p (a b) -> p a b", b=128), HY_v[k * 128:(k + 1) * 128, ci * 4:(ci + 1) * 4, :], w=[hyt], key=hyt)
            dma("sync", ga[:], Gs[:, t0:t0 + 512].rearrange("(k p) t -> p k t", p=128), w=[ga], key=ga)
            yT = yTr.next()
            for f in range(8):
                bA, bH = prot.next(), prot.next()
                for k in range(4):
                    op("tensor", lambda e, bA=bA, k=k, f=f, at=at: e.matmul(bA[:, :], lhsT=wba[:, k, f * 128:(f + 1) * 128], rhs=at[:, k, :],
                                                                         start=(k == 0), stop=(k == 3)), r=[wba, at], w=[bA], inc=(k == 3))
                for k in range(4):
                    op("tensor", lambda e, bH=bH, k=k, f=f, hyt=hyt: e.matmul(bH[:, :], lhsT=wbh[:, k, f * 128:(f + 1) * 128], rhs=hyt[:, k, :],
                                                                           start=(k == 0), stop=(k == 3)), r=[wbh, hyt], w=[bH], inc=(k == 3))
                t1, t2 = t1r.next(), t2r.next()
                op("vector", lambda e, t1=t1, bA=bA, ga=ga, f=f: e.tensor_tensor(out=t1[:], in0=bA[:, :], in1=ga[:, f, :], op=ALU.mult), r=[bA, ga], w=[t1])
                op("vector", lambda e, t2=t2, bH=bH, ga=ga, f=f: e.tensor_tensor(out=t2[:], in0=bH[:, :], in1=ga[:, 8 + f, :], op=ALU.mult), r=[bH, ga], w=[t2])
                op("gpsimd", lambda e, t1=t1, t2=t2, yT=yT, f=f: e.tensor_tensor(out=yT[:, f, :], in0=t1[:], in1=t2[:], op=ALU.add), r=[t1, t2], w=[yT])
            def tile_chain(i):
                tok = t0 + i * 128
                xt = xr.next()
                dma("sync", xt[:], x_d[tok:tok + 128, :], w=[xt], key=xt)
                xm = xmr.next()
                tm = tmr.next()
                for dh in range(2):
                    bank = prot.next()
                    for f in range(8):
                        op("tensor", lambda e, bank=bank, f=f, i=i, dh=dh, yT=yT: e.matmul(
                            bank[:, :], lhsT=yT[:, f, i * 128:(i + 1) * 128], rhs=wout[:, f, dh * 512:(dh + 1) * 512], start=(f == 0), stop=(f == 7)),
                           r=[yT, wout], w=[bank], inc=(f == 7))
                    op("vector", lambda e, tm=tm, bank=bank, dh=dh: e.tensor_tensor(out=tm[:, dh * 512:(dh + 1) * 512], in0=bank[:, :],
                                                                                  in1=G2[:, dh * 512:(dh + 1) * 512], op=ALU.mult), r=[bank, G2], w=[tm])
                op("gpsimd", lambda e, xm=xm, tm=tm, xt=xt: e.tensor_tensor(out=xm[:], in0=tm[:], in1=xt[:], op=ALU.add), r=[tm, xt], w=[xm])
                dma("sync", XMs[tok:tok + 128, :], xm[:], r=[xm], key=xm)
                yield
                jk, st = jkr.next(), sttr.next()
                op("scalar", lambda e, jk=jk, xm=xm, st=st: e.activation(out=jk[:], in_=xm[:], func=AF.Square, accum_out=st[:, 0:1]), r=[xm], w=[jk, st])
                op("scalar", lambda e, st=st: e.activation(out=st[:, 1:2], in_=st[:, 0:1], func=AF.Sqrt, scale=1.0 / D, bias=EPS), r=[st], w=[st])
                op("vector", lambda e, st=st: e.reciprocal(out=st[:, 2:3], in_=st[:, 1:2]), r=[st], w=[st])
                h2 = h2r.next()
                h2a = tmr.next()
                op("vector", lambda e, h2a=h2a, xm=xm, st=st: e.scalar_tensor_tensor(out=h2a[:], in0=xm[:], scalar=st[:, 2:3], in1=A2[:],
                                                                                    op0=ALU.mult, op1=ALU.mult), r=[xm, st, A2], w=[h2a])
                op("gpsimd", lambda e, h2=h2, h2a=h2a: e.tensor_tensor(out=h2[:], in0=h2a[:], in1=B2[:], op=ALU.add), r=[h2a, B2], w=[h2])
                yield
                h2T, h2Tb = h2Tr.next(), h2Tbr.next()
                for half in range(2):
                    bank = prot.next()
                    for kk in range(4):
                        k = half * 4 + kk
                        op("tensor", lambda e, bank=bank, kk=kk, k=k, h2=h2: e.transpose(
                            out=bank[:, kk * 128:(kk + 1) * 128], in_=h2[:, k * 128:(k + 1) * 128], identity=ident[:]),
                           r=[h2, ident], w=[bank], inc=True)
                    op("vector", lambda e, bank=bank, half=half, h2T=h2T: e.tensor_copy(
                        out=h2T[:, half * 4:half * 4 + 4, :], in_=bank[:, :].rearrange("p (k t) -> p k t", t=128)), r=[bank], w=[h2T])
                    op("gpsimd", lambda e, half=half, h2T=h2T, h2Tb=h2Tb: e.tensor_copy(
                        out=h2Tb[:, half * 4:half * 4 + 4, :], in_=h2T[:, half * 4:half * 4 + 4, :]), r=[h2T], w=[h2Tb])
                if True:
                    dma("sync", H2Ts[:, tok:tok + 128].rearrange("(k p) t -> p k t", p=128), h2Tb[:], r=[h2Tb], key=h2Tb)
                yield
                bank = prot.next()
                for k in range(8):
                    op("tensor", lambda e, bank=bank, k=k, h2T=h2T: e.matmul(bank[:, 0:NE], lhsT=h2T[:, k, :], rhs=wr[:, k, :], start=(k == 0), stop=(k == 7)),
                       r=[h2T, wr], w=[bank], inc=(k == 7))
                rt, sm = rtr.next(), smr.next()
                sc, sel, eq, sel2, selm, em, w_, wt = (rt[:, j, :] for j in range(8))
                m1, m2, gs, g8, gm, pen, e8, ws = (sm[:, 8 * j:8 * j + 8] for j in range(8))
                V = lambda fn, r_, w__: op("vector", fn, r=r_, w=w__)
                op("scalar", lambda e, sc=sc, bank=bank: e.activation(out=sc, in_=bank[:, 0:NE], func=AF.Sigmoid), r=[bank], w=[rt])
                V(lambda e, sel=sel, sc=sc: e.tensor_tensor(out=sel, in0=sc, in1=rbb[:], op=ALU.add), [rt, rbb], [rt])
                g3 = lambda a: a.rearrange("p (g e) -> p g e", e=8)
                V(lambda e, m1=m1, sel=sel: e.tensor_reduce(out=m1, in_=g3(sel), axis=AX.X, op=ALU.max), [rt], [sm])
                V(lambda e, eq=eq, sel=sel, m1=m1: e.tensor_tensor(out=g3(eq), in0=g3(sel), in1=m1.unsqueeze(2).to_broadcast([128, 8, 8]), op=ALU.is_equal), [rt, sm], [rt])
                V(lambda e, sel2=sel2, eq=eq, sel=sel: e.scalar_tensor_tensor(out=sel2, in0=eq, scalar=-BIG, in1=sel, op0=ALU.mult, op1=ALU.add), [rt], [rt])
                V(lambda e, m2=m2, sel2=sel2: e.tensor_reduce(out=m2, in_=g3(sel2), axis=AX.X, op=ALU.max), [rt], [sm])
                V(lambda e, gs=gs, m1=m1, m2=m2: e.tensor_tensor(out=gs, in0=m1, in1=m2, op=ALU.add), [sm], [sm])
                V(lambda e, g8=g8, gs=gs: e.max(out=g8, in_=gs), [sm], [sm])
                V(lambda e, gm=gm, gs=gs, g8=g8: e.tensor_scalar(out=gm, in0=gs, scalar1=g8[:, 3:4], scalar2=None, op0=ALU.is_ge), [sm], [sm])
                V(lambda e, pen=pen, gm=gm: e.tensor_scalar(out=pen, in0=gm, scalar1=-1.0, scalar2=BIG, op0=ALU.add, op1=ALU.mult), [sm], [sm])
                V(lambda e, selm=selm, sel=sel, pen=pen: e.tensor_tensor(out=g3(selm), in0=g3(sel), in1=pen.unsqueeze(2).to_broadcast([128, 8, 8]), op=ALU.add), [rt, sm], [rt])
                V(lambda e, e8=e8, selm=selm: e.max(out=e8, in_=selm), [rt], [sm])
                V(lambda e, em=em, selm=selm, e8=e8: e.tensor_scalar(out=em, in0=selm, scalar1=e8[:, 7:8], scalar2=None, op0=ALU.is_ge), [rt, sm], [rt])
                V(lambda e, w_=w_, sc=sc, em=em: e.tensor_tensor(out=w_, in0=sc, in1=em, op=ALU.mult), [rt], [rt])
                V(lambda e, ws=ws, w_=w_: e.tensor_reduce(out=ws[:, 0:1], in_=w_, axis=AX.X, op=ALU.add), [rt], [sm])
                V(lambda e, ws=ws: e.reciprocal(out=ws[:, 1:2], in_=ws[:, 0:1]), [sm], [sm])
                V(lambda e, wt=wt, w_=w_, ws=ws: e.tensor_scalar(out=wt, in0=w_, scalar1=ws[:, 1:2], scalar2=2.5, op0=ALU.mult, op1=ALU.mult), [rt, sm], [rt])
                yield
                bank = prot.next()
                op("tensor", lambda e, bank=bank, wt=wt: e.transpose(out=bank[0:NE, 0:128], in_=wt, identity=ident[:]), r=[rt, ident], w=[bank])
                wtb, wl = wtr.next(), wlr.next()
                op("scalar", lambda e, wtb=wtb, bank=bank: e.copy(out=wtb[:, 0, :], in_=bank[0:NE, 0:128]), r=[bank], w=[wtb])
                op("vector", lambda e, wl=wl, bank=bank, wtb=wtb: e.tensor_tensor(out=wl[:], in0=bank[0:NE, 0:128], in1=wtb[:, 0, :], op=ALU.subtract), r=[bank, wtb], w=[wl])
                op("gpsimd", lambda e, wl=wl, wtb=wtb: e.tensor_copy(out=wtb[:, 1, :], in_=wl[:]), r=[wl], w=[wtb])
                dma("sync", WTs[:, :, tok:tok + 128].rearrange("h e t -> e h t"), wtb[:], r=[wtb], key=wtb)
            return [tile_chain(i) for i in range(4)]

        def all_tile_chains():
            for ci in range(lim.get("merge", SEQ // 512)):
                for c_ in merge_chunk(ci):
                    yield c_
        interleave(all_tile_chains(), lim.get("mergew", 2))
        kb.end_phase()


    if "moe" in phases:
        wsg_d = kb.inp("w_sh_gate", [D, EFF]); wsu_d = kb.inp("w_sh_up", [D, EFF]); wsd_d = kb.inp("w_sh_down", [EFF, D])
        out_d = kb.nc.dram_tensor("out", [SEQ, D], F32, kind="ExternalOutput").ap()
        kb.begin_phase()
        if not converted[0]:
            convert_expert_weights()
        wsh = kb.sb("wsh", [128, 8, 2 * EFF], BF16)
        wshd = kb.sb("wshd", [128, 2, D], BF16)
        dma("gpsimd", wsh[:, :, 0:EFF], wsg_d.rearrange("(k p) f -> p k f", p=128), w=[wsh], key=wsh)
        dma("gpsimd", wsh[:, :, EFF:2 * EFF], wsu_d.rearrange("(k p) f -> p k f", p=128), w=[wsh], key=wsh)
        dma("gpsimd", wshd[:], wsd_d.rearrange("(k p) d -> p k d", p=128), w=[wshd], key=wshd)
        G5 = kb.sb("G5", [128, D], F32); FN = kb.sb("FN", [128, D], F32)
        dma("sync", G5[:], MODS[5], w=[G5], key=G5)
        dma("sync", FN[:], fn_d, w=[FN], key=FN)
        Esel = kb.sb("Esel", [NE, NE, 128], BF16)
        op("vector", lambda e: e.tensor_copy(out=Esel[:], in_=identb[0:NE, 0:NE].unsqueeze(2).to_broadcast([NE, NE, 128])), r=[identb], w=[Esel])
        S.barrier()
        GE = 4
        h2Tcr = kb.rot("h2Tc", 1, [128, 8, 1024], BF16)
        wtcr = kb.rot("wtc", 1, [NE, 1024], BF16)
        acc = kb.sb("acc", [128, 8, D], F32)
        wgur = kb.rot("wgu", 3, [128, 2, 8 * EFF], BF16)
        wdr = kb.rot("wd", 8, [128, 2 * D], BF16)
        aT = kb.sb("aT", [128, GE, 2, 1024], BF16)
        bcsr = kb.rot("bcs", 4, [128, 512], BF16)
        ssr = kb.rot("ss", 2, [128, 512], BF16)
        ttr = kb.rot("tt", 2, [128, 512], BF16)
        xmr2 = kb.rot("xm2", 2, [128, D], F32)
        fr1 = kb.rot("f1", 1, [128, D], F32)
        fr2 = kb.rot("f2", 1, [128, D], F32)
        jk2 = kb.rot("jk2", 1, [128, D], BF16)
        st2 = kb.rot("st2", 4, [128, 4], F32)
        outr = kb.rot("outt", 2, [128, D], F32)
        groups = [list(range(g0, min(g0 + GE, ne_lim))) for g0 in range(0, ne_lim, GE)] + [["sh"]]
        for ch in range(lim.get("moe", SEQ // 1024)):
            t0 = ch * 1024
            h2Tc, wtc = h2Tcr.next(), wtcr.next()
            for k in range(8):
                dma("sync", h2Tc[:, k, :], H2Ts[k * 128:(k + 1) * 128, t0:t0 + 1024], w=[h2Tc], key=h2Tc)
            dma("sync", wtc[:], WTs[0, :, t0:t0 + 1024], w=[wtc], key=wtc)
            for gi, grp in enumerate(groups):
                wts = []
                for ei, e_ in enumerate(grp):
                    if e_ == "sh":
                        wts.append((wsh, wsh[:, :, 0:EFF], wsh[:, :, EFF:2 * EFF], wshd, wshd[:]))
                    else:
                        wgu, wd = wgur.next(), wdr.next()
                        dma("sync", wgu[:, 0, :], EWg[e_], w=[wgu], key=wgu)
                        dma("sync", wgu[:, 1, :], EWu[e_], w=[wgu], key=wgu)
                        dma("sync", wd[:], EWd[e_], w=[wd], key=wd)
                        wts.append((wgu, wgu[:, 0, :].rearrange("p (k f) -> p k f", k=8), wgu[:, 1, :].rearrange("p (k f) -> p k f", k=8),
                                    wd, wd[:].rearrange("p (k d) -> p k d", k=2)))
                    wgub, wgv, wuv, wdb, wdv = wts[ei]
                    for half in range(2):
                        hs = slice(half * 512, half * 512 + 512)
                        if e_ != "sh":
                            bk = prot.next()
                            op("tensor", lambda e, bk=bk, e_=e_, wtc=wtc, hs=hs: e.matmul(bk[:, :], lhsT=Esel[:, e_, :], rhs=wtc[:, hs], start=True, stop=True),
                               r=[Esel, wtc], w=[bk])
                            bcs = bcsr.next()
                            op("scalar", lambda e, bcs=bcs, bk=bk: e.copy(out=bcs[:], in_=bk[:, :]), r=[bk], w=[bcs])
                        for f in range(2):
                            bg, bu = prot.next(), prot.next()
                            for (bank, wv) in ((bg, wgv), (bu, wuv)):
                                for k in range(8):
                                    op("tensor", lambda e, bank=bank, wv=wv, k=k, f=f, h2Tc=h2Tc, hs=hs: e.matmul(
                                        bank[:, :], lhsT=wv[:, k, f * 128:(f + 1) * 128], rhs=h2Tc[:, k, hs], start=(k == 0), stop=(k == 7)),
                                       r=[wgub, h2Tc], w=[bank], inc=(k == 7))
                            ss = ssr.next()
                            op("scalar", lambda e, ss=ss, bg=bg: e.activation(out=ss[:], in_=bg[:, :], func=AF.Silu), r=[bg], w=[ss])
                            if e_ == "sh":
                                op("vector", lambda e, bu=bu, ss=ss, ei=ei, f=f, hs=hs: e.tensor_tensor(out=aT[:, ei, f, hs], in0=bu[:, :], in1=ss[:], op=ALU.mult),
                                   r=[bu, ss], w=[aT])
                            else:
                                tt = ttr.next()
                                op("vector", lambda e, tt=tt, bu=bu, ss=ss: e.tensor_tensor(out=tt[:], in0=bu[:, :], in1=ss[:], op=ALU.mult), r=[bu, ss], w=[tt])
                                op("gpsimd", lambda e, tt=tt, bcs=bcs, ei=ei, f=f, hs=hs: e.tensor_tensor(out=aT[:, ei, f, hs], in0=tt[:], in1=bcs[:], op=ALU.mult),
                                   r=[tt, bcs], w=[aT])
                for ti in range(8):
                    for dh in range(2):
                        bank = prot.next()
                        nmm = len(grp) * 2
                        j = 0
                        for ei in range(len(grp)):
                            wdb, wdv = wts[ei][3], wts[ei][4]
                            for f in range(2):
                                op("tensor", lambda e, bank=bank, ei=ei, f=f, ti=ti, dh=dh, wdv=wdv, j=j, nmm=nmm: e.matmul(
                                    bank[:, :], lhsT=aT[:, ei, f, ti * 128:(ti + 1) * 128], rhs=wdv[:, f, dh * 512:(dh + 1) * 512],
                                    start=(j == 0), stop=(j == nmm - 1)), r=[aT, wdb], w=[bank], inc=(j == nmm - 1))
                                j += 1
                        dsl = slice(dh * 512, dh * 512 + 512)
                        if gi == 0:
                            op("scalar", lambda e, bank=bank, ti=ti, dsl=dsl: e.copy(out=acc[:, ti, dsl], in_=bank[:, :]), r=[bank], w=[acc])
                        else:
                            op("vector", lambda e, bank=bank, ti=ti, dsl=dsl: e.tensor_tensor(out=acc[:, ti, dsl], in0=bank[:, :], in1=acc[:, ti, dsl], op=ALU.add),
                               r=[bank, acc], w=[acc])
            for ti in range(8):
                tok = t0 + ti * 128
                xm = xmr2.next()
                dma("sync", xm[:], XMs[tok:tok + 128, :], w=[xm], key=xm)
                f1, f2 = fr1.next(), fr2.next()
                op("vector", lambda e, f1=f1, ti=ti: e.tensor_tensor(out=f1[:], in0=acc[:, ti, :], in1=G5[:], op=ALU.mult), r=[acc, G5], w=[f1])
                op("gpsimd", lambda e, f1=f1, f2=f2, xm=xm: e.tensor_tensor(out=f2[:], in0=f1[:], in1=xm[:], op=ALU.add), r=[f1, xm], w=[f2])
                jk, st = jk2.next(), st2.next()
                op("scalar", lambda e, jk=jk, f2=f2, st=st: e.activation(out=jk[:], in_=f2[:], func=AF.Square, accum_out=st[:, 0:1]), r=[f2], w=[jk, st])
                op("scalar", lambda e, st=st: e.activation(out=st[:, 1:2], in_=st[:, 0:1], func=AF.Sqrt, scale=1.0 / D, bias=EPS), r=[st], w=[st])
                op("vector", lambda e, st=st: e.reciprocal(out=st[:, 2:3], in_=st[:, 1:2]), r=[st], w=[st])
                ot = outr.next()
                op("vector", lambda e, ot=ot, f2=f2, st=st: e.scalar_tensor_tensor(out=ot[:], in0=f2[:], scalar=st[:, 2:3], in1=FN[:], op0=ALU.mult, op1=ALU.mult),
                   r=[f2, st, FN], w=[ot])
                dma("sync", out_d[tok:tok + 128, :], ot[:], r=[ot], key=ot)
        kb.end_phase()

    return kb


_ROPE_PERM = np.array(list(range(8, 16)) + list(range(0, 8)) + list(range(24, 32)) + list(range(16, 24)))


def _rope_tables():
    half = 16
    inv_freq = (np.float32(10000.0) ** (-np.arange(0, half, 2, dtype=np.float32) / np.float32(half))).astype(np.float32)
    rows = SEQ // 64
    row = np.broadcast_to(np.arange(rows, dtype=np.float32)[:, None], (rows, 64)).reshape(-1)
    col = np.broadcast_to(np.arange(64, dtype=np.float32)[None, :], (rows, 64)).reshape(-1)
    ang_r = (row[:, None] * inv_freq).astype(np.float32)
    ang_c = (col[:, None] * inv_freq).astype(np.float32)
    C = np.zeros((32, SEQ), np.float32)
    Sg = np.zeros((32, SEQ), np.float32)
    C[0:8] = np.cos(ang_r).T; C[8:16] = np.cos(ang_r).T
    C[16:24] = np.cos(ang_c).T; C[24:32] = np.cos(ang_c).T
    Sg[0:8] = -np.sin(ang_r).T; Sg[8:16] = np.sin(ang_r).T
    Sg[16:24] = -np.sin(ang_c).T; Sg[24:32] = np.sin(ang_c).T
    return C, Sg


def _colsT(v, n):
    return np.ascontiguousarray(np.asarray(v, np.float32).reshape(n, 128).T)


def prep_shared(inp):
    g = lambda k: np.asarray(inp[k][0], np.float32)
    out = {}
    out["cctxT"] = _colsT(inp["c_ctx"], 8)
    out["w_mod"] = g("w_mod")
    out["b_mod"] = g("b_mod")[None, :]
    out["nmix_b"] = np.ascontiguousarray(np.broadcast_to(g("norm_mix")[None, :], (128, D)))
    out["nffn_b"] = np.ascontiguousarray(np.broadcast_to(g("norm_ffn")[None, :], (128, D)))
    out["fn_b"] = np.ascontiguousarray(np.broadcast_to(np.asarray(inp["final_norm"], np.float32)[None, :], (128, D)))
    w_in = g("w_in"); b_in = g("b_in")
    out["w_in"] = w_in
    binT = np.zeros((128, 33), np.float32)
    binT[:, 0:2] = _colsT(b_in[C_Q:C_Q + 256], 2)
    binT[:, 2:3] = _colsT(b_in[C_KV:C_KV + 128], 1)
    binT[:, 3:15] = _colsT(b_in[C_HY:C_HY + 1536], 12)
    binT[:, 15:31] = _colsT(b_in[C_GATE:C_GATE + 2048], 16)
    binT[0:32, 31] = b_in[C_KPE:C_KPE + 32]
    binT[0:32, 32] = b_in[C_KPE:C_KPE + 32][_ROPE_PERM]
    out["b_inT"] = binT
    out["w_kpe_sw"] = np.ascontiguousarray(w_in[:, C_KPE:C_KPE + 32][:, _ROPE_PERM])
    C, Sg = _rope_tables()
    out["ropeC"] = C; out["ropeS"] = Sg
    out["q_normT"] = _colsT(g("q_norm"), 2)
    out["kv_normT"] = _colsT(g("kv_norm"), 1)
    w_uq = g("w_uq")
    out["w_uq"] = w_uq
    wsw = w_uq.reshape(256, H, DQ).copy()
    wsw[:, :, 64:96] = wsw[:, :, 64:96][:, :, _ROPE_PERM]
    out["w_uq_sw"] = np.ascontiguousarray(wsw.reshape(256, H * DQ))
    wkv = g("w_ukv").reshape(128, H, 128)
    out["w_ukv_k"] = np.ascontiguousarray(wkv[:, :, 0:64].reshape(128, 512))
    out["w_ukv_v"] = np.ascontiguousarray(wkv[:, :, 64:128].reshape(128, 512))
    hcw = g("hy_conv_w")
    t = np.zeros((128, 36), np.float32)
    for j in range(12):
        for k in range(3):
            t[:, 3 * j + k] = hcw[k, j * 128:(j + 1) * 128]
    out["hy_conv_wT"] = t
    out["hy_conv_bT"] = _colsT(g("hy_conv_b"), 12)
    out["ident"] = np.eye(128, dtype=np.float32)
    jk = np.outer(np.arange(128), np.arange(128)).astype(np.float64)
    out["dftC"] = np.cos(2 * np.pi * jk / 128).astype(np.float32)
    out["dftS"] = np.sin(2 * np.pi * jk / 128).astype(np.float32)
    out["twC"] = np.cos(2 * np.pi * jk / NFFT).astype(np.float32)
    out["twS"] = np.sin(2 * np.pi * jk / NFFT).astype(np.float32)
    L = SEQ
    t = np.linspace(0.0, 1.0, L, dtype=np.float32)
    w = (np.float32(2.0 * math.pi) * np.arange(L, dtype=np.float32) / np.float32(L)).astype(np.float32)
    f = np.linspace(1e-4, 7.0, 8, dtype=np.float32)[None, :]
    fw = (f * w[:, None]).astype(np.float32)
    emb = np.concatenate([t[:, None], np.cos(fw), -np.sin(fw)], axis=-1).astype(np.float32)
    ridx = np.concatenate([[0], L - np.arange(1, L)])
    out["embF"] = np.ascontiguousarray(emb.T)
    out["embB"] = np.ascontiguousarray(emb[np.minimum(ridx, L - 1)].T)
    tB = t[np.minimum(ridx, L - 1)].copy(); tB[0] = 1e4
    out["trowF"] = np.ascontiguousarray(np.broadcast_to(t[None, :], (128, L)))
    out["trowB"] = np.ascontiguousarray(np.broadcast_to(tB[None, :], (128, L)))
    deltas = np.abs(np.linspace(math.log(1e-2) / 1.5, math.log(1e-2) / 0.3, HYW, dtype=np.float32))
    out["negdelta"] = np.ascontiguousarray(-deltas.reshape(4, 128).T)
    out["hf_w1"] = g("hy_filt_w1"); out["hf_w2"] = g("hy_filt_w2"); out["hf_w3"] = g("hy_filt_w3")
    out["hf_vecs"] = np.ascontiguousarray(np.stack([g("hy_filt_b1"), g("hy_filt_b2"), g("hy_filt_freq")], axis=1))
    out["skip_b"] = np.ascontiguousarray(np.broadcast_to(g("hy_skip")[None], (128, 2, HYW)))
    out["w_ba"] = g("w_branch_attn"); out["w_bh"] = g("w_branch_hyena"); out["w_out"] = g("w_out")
    out["w_router"] = g("w_router")
    out["rbias_b"] = np.ascontiguousarray(np.broadcast_to(g("router_bias")[None], (128, NE)))
    out["w_exp_gate"] = g("w_exp_gate"); out["w_exp_up"] = g("w_exp_up"); out["w_exp_down"] = g("w_exp_down")
    out["w_sh_gate"] = g("w_sh_gate"); out["w_sh_up"] = g("w_sh_up"); out["w_sh_down"] = g("w_sh_down")
    return out


def prep_core(inp, b):
    return {
        "x": np.ascontiguousarray(np.asarray(inp["x"][b], np.float32)),
        "ctx": np.ascontiguousarray(np.asarray(inp["ctx"][b], np.float32)),
        "cT": _colsT(inp["c"][b], 8),
    }


_PROGRAM = None


def kernel(**inputs):
    global _PROGRAM
    if _PROGRAM is None:
        _PROGRAM = build()
    kb = _PROGRAM
    shared = prep_shared(inputs)
    in_maps = []
    for b in range(8):
        allin = dict(shared)
        allin.update(prep_core(inputs, b))
        in_maps.append({k: allin[k] for k in kb.inputs})
    res = run_bass_kernel_spmd(kb.nc, in_maps, core_ids=list(range(8)))
    out = np.stack([np.asarray(r["out"], dtype=np.float32) for r in res.results], axis=0)
    return out
```
